# Optimizing a Trainium2 kernel written in Bass

```python
import math
import jax, jax.numpy as jnp
from jax import lax
import numpy as np

D_MODEL = 1024
BATCH = 32
SEQ = 2048
DEPTH = 1

HEAD_DIM = 64
DIFF_HEADS = D_MODEL // (4 * HEAD_DIM)
DIFF_QK_WIDTH = DIFF_HEADS * 2 * HEAD_DIM
DIFF_V_DIM = 2 * HEAD_DIM
DIFF_WIDTH = DIFF_HEADS * DIFF_V_DIM
SWA_Q_HEADS = D_MODEL // (2 * HEAD_DIM)
SWA_KV_HEADS = 2
SWA_GROUP = SWA_Q_HEADS // SWA_KV_HEADS
SWA_WIDTH = SWA_Q_HEADS * HEAD_DIM
SWA_KV_WIDTH = SWA_KV_HEADS * HEAD_DIM
WINDOW = 128
BLOCK = 128
MIX_WIDTH = DIFF_WIDTH + SWA_WIDTH
IN_WIDTH = 2 * DIFF_QK_WIDTH + DIFF_WIDTH + SWA_WIDTH + 2 * SWA_KV_WIDTH
SPLIT_POINTS = [DIFF_QK_WIDTH,
                2 * DIFF_QK_WIDTH,
                2 * DIFF_QK_WIDTH + DIFF_WIDTH,
                2 * DIFF_QK_WIDTH + DIFF_WIDTH + SWA_WIDTH,
                2 * DIFF_QK_WIDTH + DIFF_WIDTH + SWA_WIDTH + SWA_KV_WIDTH]
ROPE_THETA = 10000.0
N_GROUPS = 4
EXPERTS_PER_GROUP = 4
N_EXPERTS = N_GROUPS * EXPERTS_PER_GROUP
TOP_K = 2
D_EXPERT = 512
EPS = 1e-6

kernel_name = "hymba_diffattn_swa_sink_hmoe_adaln"


def rmsnorm(x, g):
    xf = x.astype(jnp.float32)
    y = xf * lax.rsqrt(jnp.mean(xf * xf, axis=-1, keepdims=True) + EPS)
    return (y * g.astype(jnp.float32)).astype(x.dtype)


def rope_tables(seq, dtype):
    inv = ROPE_THETA ** (-jnp.arange(0, HEAD_DIM, 2, dtype=jnp.float32) / HEAD_DIM)
    ang = jnp.arange(seq, dtype=jnp.float32)[:, None] * inv[None, :]
    ang = jnp.concatenate([ang, ang], axis=-1)
    return jnp.cos(ang).astype(dtype), jnp.sin(ang).astype(dtype)


def apply_rope(x, cos, sin):
    shape = (1, x.shape[1]) + (1,) * (x.ndim - 3) + (HEAD_DIM,)
    cs, sn = cos.reshape(shape), sin.reshape(shape)
    x1, x2 = jnp.split(x, 2, axis=-1)
    return x * cs + jnp.concatenate([-x2, x1], axis=-1) * sn


def diff_attention(q, k, v, lam):
    B, S, H = q.shape[:3]
    nb = S // BLOCK
    scale = HEAD_DIM ** -0.5
    qb = q.reshape(B, nb, BLOCK, H, 2, HEAD_DIM).transpose(1, 0, 2, 3, 4, 5)
    kpos = jnp.arange(S)

    def one_block(args):
        qblk, n = args
        s = jnp.einsum('bqhmd,bkhmd->bhmqk', qblk, k).astype(jnp.float32) * scale
        qpos = n * BLOCK + jnp.arange(BLOCK)
        causal = kpos[None, :] <= qpos[:, None]
        p = jax.nn.softmax(jnp.where(causal, s, -jnp.inf), axis=-1)
        a = (p[:, :, 0] - lam * p[:, :, 1]).astype(v.dtype)
        return jnp.einsum('bhqk,bkhe->bqhe', a, v)

    out = lax.map(one_block, (qb, jnp.arange(nb)))
    return out.transpose(1, 0, 2, 3, 4).reshape(B, S, H, DIFF_V_DIM)


def swa_attention(q, k, v, sinks):
    B, S = q.shape[:2]
    nb = S // BLOCK
    scale = HEAD_DIM ** -0.5
    qb = q.reshape(B, nb, BLOCK, SWA_KV_HEADS, SWA_GROUP, HEAD_DIM)

    def band(t):
        tb = t.reshape(B, nb, BLOCK, SWA_KV_HEADS, HEAD_DIM)
        prev = jnp.concatenate([jnp.zeros_like(tb[:, :1]), tb[:, :-1]], axis=1)
        return jnp.concatenate([prev, tb], axis=2)

    kb, vb = band(k), band(v)
    s = jnp.einsum('bnqhgd,bnkhd->bnhgqk', qb, kb).astype(jnp.float32) * scale
    i = jnp.arange(BLOCK)[:, None]
    j = jnp.arange(2 * BLOCK)[None, :]
    dist = i + BLOCK - j
    in_window = (dist >= 0) & (dist < WINDOW)
    blk = jnp.arange(nb)[:, None, None]
    mask = in_window[None] & ((blk > 0) | (j >= BLOCK)[None])
    s = jnp.where(mask[None, :, None, None], s, -jnp.inf)
    sink = sinks.astype(jnp.float32).reshape(SWA_KV_HEADS, SWA_GROUP)[None, None, :, :, None, None]
    s_full = jnp.concatenate([s, jnp.broadcast_to(sink, s.shape[:-1] + (1,))], axis=-1)
    p = jax.nn.softmax(s_full, axis=-1)[..., :-1].astype(v.dtype)
    out = jnp.einsum('bnhgqk,bnkhd->bnqhgd', p, vb)
    return out.reshape(B, S, SWA_WIDTH)


def hierarchical_moe(h, w_rg, b_rg, w_re, b_re, w_gate, w_up, w_down):
    B, S, _ = h.shape
    grp_logits = (h @ w_rg).astype(jnp.float32) + b_rg.astype(jnp.float32)
    grp_prob = jax.nn.softmax(grp_logits, axis=-1)
    grp_p, grp_idx = lax.top_k(grp_prob, 1)
    exp_logits = ((h @ w_re).astype(jnp.float32) + b_re.astype(jnp.float32)).reshape(
        B, S, N_GROUPS, EXPERTS_PER_GROUP)
    in_grp = jnp.take_along_axis(exp_logits, grp_idx[..., None], axis=2)[:, :, 0]
    exp_prob = jax.nn.softmax(in_grp, axis=-1)
    top_p, top_i = lax.top_k(exp_prob, TOP_K)
    top_p = top_p / jnp.sum(top_p, axis=-1, keepdims=True)
    within = jnp.sum(jax.nn.one_hot(top_i, EXPERTS_PER_GROUP, dtype=jnp.float32) * top_p[..., None], axis=-2)
    grp_w = jax.nn.one_hot(grp_idx[..., 0], N_GROUPS, dtype=jnp.float32) * grp_p
    combine = (grp_w[..., :, None] * within[..., None, :]).reshape(B, S, N_EXPERTS).astype(h.dtype)
    y = jnp.zeros_like(h)
    for e in range(N_EXPERTS):
        a = jax.nn.silu(h @ w_gate[e]) * (h @ w_up[e])
        y = y + combine[..., e:e + 1] * (a @ w_down[e])
    return y


def setup_inputs(seed: int = 0) -> dict:
    key = jax.random.key(seed)
    ks = jax.random.split(key, 20)
    f32 = jnp.float32
    D = D_MODEL
    nrm = lambda k, shape, scale: jax.random.normal(k, shape, f32) * scale
    return {
        "x": nrm(ks[0], (BATCH, SEQ, D), 1.0),
        "c": nrm(ks[1], (BATCH, D), 1.0),
        "w_ada": nrm(ks[2], (DEPTH, D, 6 * D), D ** -0.5),
        "b_ada": nrm(ks[3], (DEPTH, 6 * D), 0.02),
        "g_mix": 1.0 + nrm(ks[4], (DEPTH, D), 0.02),
        "w_in": nrm(ks[5], (DEPTH, D, IN_WIDTH), D ** -0.5),
        "diff_lambda": nrm(ks[6], (DEPTH, 4, HEAD_DIM), 0.1),
        "g_diff_sub": 1.0 + nrm(ks[7], (DEPTH, DIFF_V_DIM), 0.02),
        "swa_sinks": nrm(ks[8], (DEPTH, SWA_Q_HEADS), 0.5),
        "w_out": nrm(ks[9], (DEPTH, MIX_WIDTH, D), MIX_WIDTH ** -0.5),
        "g_ffn": 1.0 + nrm(ks[10], (DEPTH, D), 0.02),
        "w_route_group": nrm(ks[11], (DEPTH, D, N_GROUPS), D ** -0.5),
        "b_route_group": nrm(ks[12], (DEPTH, N_GROUPS), 0.01),
        "w_route_expert": nrm(ks[13], (DEPTH, D, N_EXPERTS), D ** -0.5),
        "b_route_expert": nrm(ks[14], (DEPTH, N_EXPERTS), 0.01),
        "w_gate": nrm(ks[15], (DEPTH, N_EXPERTS, D, D_EXPERT), D ** -0.5),
        "w_up": nrm(ks[16], (DEPTH, N_EXPERTS, D, D_EXPERT), D ** -0.5),
        "w_down": nrm(ks[17], (DEPTH, N_EXPERTS, D_EXPERT, D), D_EXPERT ** -0.5),
        "g_final": 1.0 + nrm(ks[18], (D,), 0.02),
    }


def reference(x, c, w_ada, b_ada, g_mix, w_in, diff_lambda, g_diff_sub, swa_sinks, w_out,
              g_ffn, w_route_group, b_route_group, w_route_expert, b_route_expert,
              w_gate, w_up, w_down, g_final):
    B, S, _ = x.shape
    cos, sin = rope_tables(S, x.dtype)
    c_act = jax.nn.silu(c)
    for l in range(DEPTH):
        mod = c_act @ w_ada[l] + b_ada[l]
        sh1, sc1, gt1, sh2, sc2, gt2 = [m[:, None, :] for m in jnp.split(mod, 6, axis=-1)]

        h = rmsnorm(x, g_mix[l]) * (1.0 + sc1) + sh1
        proj = h @ w_in[l]
        dq, dk, dv, sq, sk, sv = jnp.split(proj, SPLIT_POINTS, axis=-1)

        lambda_init = 0.8 - 0.6 * math.exp(-0.3 * l)
        lam_p = diff_lambda[l].astype(jnp.float32)
        lam = jnp.exp(jnp.sum(lam_p[0] * lam_p[1])) - jnp.exp(jnp.sum(lam_p[2] * lam_p[3])) + lambda_init
        dq = apply_rope(dq.reshape(B, S, DIFF_HEADS, 2, HEAD_DIM), cos, sin)
        dk = apply_rope(dk.reshape(B, S, DIFF_HEADS, 2, HEAD_DIM), cos, sin)
        dv = dv.reshape(B, S, DIFF_HEADS, DIFF_V_DIM)
        o_diff = diff_attention(dq, dk, dv, lam)
        o_diff = (rmsnorm(o_diff, g_diff_sub[l]) * (1.0 - lambda_init)).reshape(B, S, DIFF_WIDTH)

        sq = apply_rope(sq.reshape(B, S, SWA_Q_HEADS, HEAD_DIM), cos, sin)
        sk = apply_rope(sk.reshape(B, S, SWA_KV_HEADS, HEAD_DIM), cos, sin)
        sv = sv.reshape(B, S, SWA_KV_HEADS, HEAD_DIM)
        o_swa = swa_attention(sq, sk, sv, swa_sinks[l])

        mix = jnp.concatenate([o_diff, o_swa], axis=-1) @ w_out[l]
        x = x + gt1 * mix

        h2 = rmsnorm(x, g_ffn[l]) * (1.0 + sc2) + sh2
        y = hierarchical_moe(h2, w_route_group[l], b_route_group[l], w_route_expert[l],
                             b_route_expert[l], w_gate[l], w_up[l], w_down[l])
        x = x + gt2 * y
    return rmsnorm(x, g_final)
```

```python
import math
import types
from contextlib import ExitStack

import numpy as np
import ml_dtypes

import concourse.bass as bass
import concourse.mybir as mybir
from concourse.bass_utils import run_bass_kernel_spmd

F32 = mybir.dt.float32
BF16 = mybir.dt.bfloat16
AF = mybir.ActivationFunctionType
ALU = mybir.AluOpType
AX = mybir.AxisListType

D = 1024
NCORES = 8
EPS = 1e-6
SB_BASE = 16512
SB_LIMIT = 229376
KB = 1024


def _freeze(fn):
    if fn.__closure__ is None:
        return fn
    cells = []
    for c in fn.__closure__:
        try:
            cells.append(types.CellType(c.cell_contents))
        except ValueError:
            cells.append(c)
    return types.FunctionType(fn.__code__, fn.__globals__, fn.__name__, fn.__defaults__, tuple(cells))


class Sem:
    def __init__(self, h):
        self.h = h
        self.v = 0


class Eng:
    def __init__(self, name, sem, is_pe=False):
        self.name = name
        self.sem = sem
        self.seen = {}
        self.q = []
        self.is_pe = is_pe


class Buf:
    __slots__ = ("name", "w", "r", "dsem")

    def __init__(self, name, reg):
        self.name = name
        self.w = {}
        self.r = {}
        self.dsem = None
        reg.append(self)


class Sched:
    def __init__(self, nc, stack):
        self.nc = nc
        self.stack = stack
        self.bufs = []
        self.dma_sems = []
        self.nsem = 0
        self.pe = Eng("pe", self.new_sem("pe"), is_pe=True)
        self.act = Eng("act", self.new_sem("act"))
        self.dve = Eng("dve", self.new_sem("dve"))
        self.pool = Eng("pool", self.new_sem("pool"))
        self.sp = Eng("sp", self.new_sem("sp"))
        self.engs = [self.pe, self.act, self.dve, self.pool, self.sp]

    def new_sem(self, name):
        self.nsem += 1
        h = self.stack.enter_context(self.nc.semaphore(f"s{self.nsem}_{name}"))
        return Sem(h)

    def new_dma_sem(self, name):
        s = self.new_sem(name)
        self.dma_sems.append(s)
        return s

    def buf(self, name):
        return Buf(name, self.bufs)

    def _waits(self, E, deps):
        for s, v in deps.items():
            if s is E.sem:
                if E.is_pe or v > s.v:
                    continue
            if E.seen.get(s, 0) >= v:
                continue
            assert v <= s.v, f"wait on pending count {E.name} {v} > {s.v}"
            E.q.append(lambda eng, h=s.h, v=v: eng.wait_ge(h, v))
            E.seen[s] = v

    @staticmethod
    def _deps(reads, writes):
        deps = {}
        for b in reads:
            for s, v in b.w.items():
                if v > deps.get(s, 0):
                    deps[s] = v
        for b in writes:
            for s, v in b.w.items():
                if v > deps.get(s, 0):
                    deps[s] = v
            for s, v in b.r.items():
                if v > deps.get(s, 0):
                    deps[s] = v
        return deps

    def op(self, E, fn, reads=(), writes=(), inc=True):
        fn = _freeze(fn)
        self._waits(E, self._deps(reads, writes))
        if inc:
            E.sem.v += 1
            val = E.sem.v
            E.q.append(lambda eng, fn=fn, h=E.sem.h: fn(eng).then_inc(h, 1))
        else:
            val = E.sem.v + 1
            E.q.append(lambda eng, fn=fn: fn(eng))
        s = E.sem
        for b in reads:
            if b.r.get(s, 0) < val:
                b.r[s] = val
        for b in writes:
            if b.w.get(s, 0) < val:
                b.w[s] = val

    def dma(self, Q, out, in_, sem, reads=(), writes=()):
        if sem is None:
            wb = writes[0]
            if wb.dsem is None:
                wb.dsem = self.new_dma_sem("d_" + wb.name)
            sem = wb.dsem
        self._waits(Q, self._deps(reads, writes))
        sem.v += 16
        Q.q.append(lambda eng, out=out, in_=in_, h=sem.h: eng.dma_start(out=out, in_=in_).then_inc(h, 16))
        for b in reads:
            b.r[sem] = sem.v
        for b in writes:
            b.w[sem] = sem.v

    def barrier(self, new_sems=False):
        allsems = [E.sem for E in self.engs[:4]] + self.dma_sems
        for E in self.engs:
            deps = {s: s.v for s in allsems if s.v > 0 and s is not E.sem}
            self._waits(E, deps)
        for b in self.bufs:
            b.w.clear()
            b.r.clear()
        if new_sems:
            for E in self.engs[:4]:
                E.sem = self.new_sem(E.name)

    def finish(self, final_waits):
        for E, s in final_waits:
            self._waits(E, {s: s.v})
        with self.nc.Block() as block:
            @block.tensor
            def _(e):
                for f in self.pe.q:
                    f(e)

            @block.scalar
            def _(e):
                for f in self.act.q:
                    f(e)

            @block.vector
            def _(e):
                for f in self.dve.q:
                    f(e)

            @block.gpsimd
            def _(e):
                for f in self.pool.q:
                    f(e)

            @block.sync
            def _(e):
                for f in self.sp.q:
                    f(e)


class Region:
    def __init__(self, nc, start, end, tag):
        self.nc = nc
        self.off = start
        self.end = end
        self.tag = tag

    def alloc(self, name, shape, dt):
        esz = 4 if dt == F32 else 2
        nbytes = int(np.prod(shape[1:])) * esz
        o = self.off
        self.off += (nbytes + 63) // 64 * 64
        assert self.off <= self.end, f"SBUF region {self.tag} overflow at {name}: {self.off} > {self.end}"
        return self.nc.alloc_sbuf_tensor_at(f"{self.tag}_{name}", shape, dt, offset=o)


class _Stop(Exception):
    pass


STOP = None


def build_nc(NSEQ, S):
    NT = S // 128
    NB = S // 512
    TP = min(S, 1024)
    NPASS = S // TP
    NTP = TP // 128
    NBP = TP // 512
    NE = 16

    nc = bass.Bass("TRN2", target_bir_lowering=False)

    def din(name, shape, dt=F32):
        return nc.dram_tensor(name, shape, dt, kind="ExternalInput").ap()

    x = din("x", [NSEQ, S, D])
    cT = din("cT", [128, 8, NSEQ])
    w_ada = din("w_ada", [D, 6 * D])
    b_ada = din("b_ada", [1, 6 * D])
    g_mix = din("g_mix", [1, D])
    g_ffn = din("g_ffn", [1, D])
    g_fin = din("g_fin", [1, D])
    w_in = din("w_in", [D, 2688])
    dlam = din("dlam", [1, 256])
    gds_c = din("gds_c", [128, 1])
    sink_c = din("sink_c", [128, 4])
    w_out = din("w_out", [D, D])
    w_r = din("w_r", [D, 20])
    b_r = din("b_r", [1, 20])
    w_gate = din("w_gate", [NE, D, 512])
    w_up = din("w_up", [NE, D, 512])
    w_down = din("w_down", [NE, 512, D])
    c_ident = din("c_ident", [128, 128])
    c_rt = din("c_rt", [128, 128])
    c_tri = din("c_tri", [128, 128])
    c_swam = din("c_swam", [128, 256])
    c_cos = din("c_cos", [128, S], BF16)
    c_sin = din("c_sin", [128, S], BF16)
    out = nc.dram_tensor("out", [NSEQ, S, D], F32, kind="ExternalOutput").ap()
    mod_scr = nc.dram_tensor("mod_scr", [NSEQ, 6 * D], F32).ap()
    wsc_g = nc.dram_tensor("wsc_g", [NE, 128, 4096], BF16).ap()
    wsc_u = nc.dram_tensor("wsc_u", [NE, 128, 4096], BF16).ap()
    wsc_d = nc.dram_tensor("wsc_d", [NE, 128, 4096], BF16).ap()

    with ExitStack() as st:
        K = Sched(nc, st)
        PE, ACT, DVE, POOL, SP = K.pe, K.act, K.dve, K.pool, K.sp

        P0 = SB_BASE
        OW0 = P0 + 29 * KB
        X0 = OW0 + 48 * KB
        rP = Region(nc, P0, OW0, "P")
        ident_f = rP.alloc("ident_f", [128, 128], F32)
        ident_b = rP.alloc("ident_b", [128, 128], BF16)
        rt_b = rP.alloc("rt_b", [128, 128], BF16)
        tri_b = rP.alloc("tri_b", [128, 128], BF16)
        swam_b = rP.alloc("swam_b", [128, 256], BF16)
        ones_b = rP.alloc("ones_b", [128, 128], BF16)
        ones_f = rP.alloc("ones_f", [128, 128], F32)
        gfin_bc = rP.alloc("gfin_bc", [128, D], F32)
        wr_f = rP.alloc("wr_f", [128, 8, 20], F32)
        br_f = rP.alloc("br_f", [128, 20], F32)
        gds = rP.alloc("gds", [128, 1], F32)
        neglam = rP.alloc("neglam", [128, 1], F32)
        esink = rP.alloc("esink", [128, 4], F32)
        epsc = rP.alloc("epsc", [128, 1], F32)
        cact = rP.alloc("cact", [128, 8, NSEQ], F32)
        lamw = rP.alloc("lamw", [128, 256], F32)
        lamt = rP.alloc("lamt", [128, 8], F32)
        stat = rP.alloc("stat", [128, 64], F32)
        A1 = rP.alloc("A1", [128, D], F32)
        B1 = rP.alloc("B1", [128, D], F32)
        A2 = rP.alloc("A2", [128, D], F32)
        B2 = rP.alloc("B2", [128, D], F32)
        G2 = rP.alloc("G2", [128, D], F32)

        rOW = Region(nc, OW0, X0, "OW")
        oT = rOW.alloc("oT", [128, 8, S], BF16)
        wob = rOW.alloc("wob", [128, 8, D], BF16)

        rS1 = Region(nc, X0, SB_LIMIT, "S1")
        hT = rS1.alloc("hT", [128, 8, S], BF16)
        xs = [rS1.alloc(f"xs{i}", [128, D], F32) for i in range(3)]
        tmpf = rS1.alloc("tmpf", [128, D], F32)
        tmpfb = rS1.alloc("tmpfb", [128, D], F32)
        hb = rS1.alloc("hb", [128, D], BF16)
        hbb = rS1.alloc("hbb", [128, D], BF16)
        junk = rS1.alloc("junk", [128, D], BF16)
        rPro = Region(nc, X0, SB_LIMIT, "Pro")
        adst = [rPro.alloc(f"adst{i}", [128, 8, 512], F32) for i in range(2)]
        bst = [rPro.alloc(f"bst{i}", [128, 512], F32) for i in range(2)]
        modsb = [rPro.alloc(f"modsb{i}", [128, 512], F32) for i in range(2)]
        cstg = rPro.alloc("cstg", [128, 256], F32)


        rS2 = Region(nc, X0, SB_LIMIT, "S2")
        rS2.off += 8 * S * 2
        hT2 = hT
        dv = rS2.alloc("dv", [128, NT, 512], BF16)
        svd = rS2.alloc("svd", [128, NT, 2, 128], BF16)
        wstg = [rS2.alloc(f"wstg{i}", [128, 8, 256], F32) for i in range(2)]
        wub = [rS2.alloc(f"wub{i}", [128, 8, 256], BF16) for i in range(2)]
        qT = rS2.alloc("qT", [128, S], BF16)
        kz = [rS2.alloc(f"kz{i}", [128, S], BF16) for i in range(2)]
        cosb = rS2.alloc("cosb", [128, S], BF16)
        sinb = rS2.alloc("sinb", [128, S], BF16)
        pT = [rS2.alloc(f"pT{i}", [128, 512], BF16) for i in range(4)]
        qraw_s = [rS2.alloc(f"qraw{i}", [128, 512], BF16) for i in range(3)]
        rt1_s = [rS2.alloc(f"rt1_{i}", [128, 512], F32) for i in range(3)]
        rt2_s = [rS2.alloc(f"rt2_{i}", [128, 512], F32) for i in range(3)]
        tA = rS2.alloc("tA", [128, 512], F32)
        tB = rS2.alloc("tB", [128, 512], F32)
        tC = rS2.alloc("tC", [128, 512], F32)
        tD = rS2.alloc("tD", [128, 512], F32)
        sqb = rS2.alloc("sqb", [128, 512], BF16)
        svsb = rS2.alloc("svsb", [128, 128], BF16)

        rM = Region(nc, X0, SB_LIMIT, "M")
        acc = rM.alloc("acc", [128, NT, D], F32)
        xs5 = [rM.alloc(f"xs5_{i}", [128, D], F32) for i in range(2)]
        wostg = [rM.alloc(f"wostg{i}", [128, 2, D], F32) for i in range(2)]
        g1t = rM.alloc("g1t", [128, D], F32)
        rMo = Region(nc, OW0, X0, "Mo")
        wgb = [rMo.alloc(f"wgb{i}", [128, 8, 512], BF16) for i in range(2)]
        wubx = [rMo.alloc(f"wubx{i}", [128, 8, 512], BF16) for i in range(2)]
        wdb = [rMo.alloc(f"wdb{i}", [128, 4, D], BF16) for i in range(2)]
        rM2 = Region(nc, X0 + NT * D * 4, SB_LIMIT, "M2")
        h2T = rM2.alloc("h2T", [128, 8, TP], BF16)
        h2f = rM2.alloc("h2f", [128, D], F32)
        tmpf2 = rM2.alloc("tmpf2", [128, D], F32)
        h2Tf = [rM2.alloc(f"h2Tf{i}", [128, 8, 128], F32) for i in range(2)]
        junk2 = rM2.alloc("junk2", [128, D], BF16)
        ytmp = [rM2.alloc(f"ytmp{i}", [128, D], F32) for i in range(2)]
        estg = [rM2.alloc(f"estg{i}", [128, 1024], F32) for i in range(2)]
        aT = [rM2.alloc(f"aT{i}", [128, 512], BF16) for i in range(8)]
        sg = [rM2.alloc(f"sg{i}", [128, 512], BF16) for i in range(2)]
        lg = rM2.alloc("lg", [128, NTP, 20], F32)
        comb = rM2.alloc("comb", [128, NTP, 16], F32)
        rw = rM2.alloc("rw", [128, 16, NTP, 4], F32)
        rs = rM2.alloc("rs", [128, 16, NTP], F32)

        ps = [st.enter_context(nc.psum_tensor(f"ps{i}", [128, 1024], F32)) for i in range(4)]
        PB = [K.buf(f"pb{i}") for i in range(8)]

        def bank(i):
            return ps[i // 2][:, (i % 2) * 512:(i % 2 + 1) * 512]

        def mk(*names):
            return {n: K.buf(n) for n in names}

        b = mk("ident_f", "ident_b", "rt_b", "tri_b", "swam_b", "ones_b", "ones_f", "gfin_bc", "wr_f", "br_f",
               "gds", "neglam", "esink", "cact", "lamw", "lamt", "stat", "A1", "B1", "A2", "B2", "G2",
               "wob", "hT", "tmpf", "hb", "junk", "cstg", "dv", "svd", "qT", "kz0", "kz1", "cosb", "sinb",
               "qraw", "rt1", "rt2", "tA", "tB", "tC", "tD", "epsc", "sqb", "svsb", "g1t", "h2T", "h2f", "tmpf2", "junk2",
               "lg", "comb", "rw", "rs", "mod_scr")
        b_xs = [K.buf(f"xs{i}") for i in range(3)]
        b_tmpf2s = [b["tmpf"], K.buf("tmpfb")]
        b_hb2s = [b["hb"], K.buf("hbb")]
        b_st = [K.buf(f"st{i}") for i in range(8)]
        b_adst = [K.buf(f"adst{i}") for i in range(2)]
        b_bst = [K.buf(f"bst{i}") for i in range(2)]
        b_modsb = [K.buf(f"modsb{i}") for i in range(2)]
        b_wstg = [K.buf(f"wstg{i}") for i in range(2)]
        b_qraw = [K.buf(f"qraw{i}") for i in range(3)]
        b_rt1 = [K.buf(f"rt1_{i}") for i in range(3)]
        b_rt2 = [K.buf(f"rt2_{i}") for i in range(3)]
        b_wub = [K.buf(f"wub{i}") for i in range(2)]
        b_pT = [K.buf(f"pT{i}") for i in range(4)]
        b_oT = [K.buf(f"oT{i}") for i in range(8)]
        b_acc = [K.buf(f"acc{i}") for i in range(NT)]
        b_xs5 = [K.buf(f"xs5_{i}") for i in range(2)]
        b_wostg = [K.buf(f"wostg{i}") for i in range(2)]
        b_wgb = [K.buf(f"wgb{i}") for i in range(2)]
        b_wubx = [K.buf(f"wubx{i}") for i in range(2)]
        b_wdb = [K.buf(f"wdb{i}") for i in range(2)]
        b_h2Tf = [K.buf(f"h2Tf{i}") for i in range(2)]
        b_ytmp = [K.buf(f"ytmp{i}") for i in range(2)]
        b_estg = [K.buf(f"estg{i}") for i in range(2)]
        b_wsc = [K.buf(f"wsc{i}") for i in range(NE)]
        b_aT = [K.buf(f"aT{i}") for i in range(8)]
        b_sg = [K.buf(f"sg{i}") for i in range(2)]

        s_const = K.new_dma_sem("const")
        s_x = [K.new_dma_sem(f"x{i}") for i in range(3)]
        s_w = [K.new_dma_sem(f"w{i}") for i in range(2)]
        s_mod = K.new_dma_sem("mod")
        s_misc = K.new_dma_sem("misc")
        s_out = K.new_dma_sem("out")

        def _ck(name):
            if STOP == name:
                raise _Stop()

        try:
            def load_const_cast(dst, dst_buf, src, width):
                K.dma(SP, cstg[:, 0:width], src, None, writes=[b["cstg"]])
                K.op(DVE, lambda e: e.tensor_copy(out=dst[:], in_=cstg[:, 0:width]), reads=[b["cstg"]], writes=[dst_buf])

            K.dma(SP, ident_f[:], c_ident, None, writes=[b["ident_f"]])
            K.op(DVE, lambda e: e.tensor_copy(out=ident_b[:], in_=ident_f[:]), reads=[b["ident_f"]], writes=[b["ident_b"]])
            load_const_cast(rt_b, b["rt_b"], c_rt, 128)
            load_const_cast(tri_b, b["tri_b"], c_tri, 128)
            load_const_cast(swam_b, b["swam_b"], c_swam, 256)
            K.op(POOL, lambda e: e.memset(ones_b[:], 1.0), writes=[b["ones_b"]])
            K.op(POOL, lambda e: e.memset(ones_f[:], 1.0), writes=[b["ones_f"]])
            K.op(POOL, lambda e: e.memset(epsc[:], EPS), writes=[b["epsc"]])
            K.dma(SP, gfin_bc[:], g_fin.partition_broadcast(128), None, writes=[b["gfin_bc"]])
            K.dma(SP, wr_f[:], w_r.rearrange("(k p) n -> p k n", p=128), None, writes=[b["wr_f"]])
            K.dma(SP, br_f[:], b_r.partition_broadcast(128), None, writes=[b["br_f"]])
            K.dma(SP, gds[:], gds_c, None, writes=[b["gds"]])
            K.dma(SP, esink[:], sink_c, None, writes=[b["esink"]])
            K.dma(SP, cact[:], cT, None, writes=[b["cact"]])
            K.dma(SP, lamw[:], dlam.partition_broadcast(128), None, writes=[b["lamw"]])

            K.op(DVE, lambda e: e.tensor_scalar(out=gds[:], in0=gds[:], scalar1=0.8, scalar2=None, op0=ALU.mult),
                 reads=[b["gds"]], writes=[b["gds"]])
            K.op(ACT, lambda e: e.activation(out=esink[:], in_=esink[:], func=AF.Exp), reads=[b["esink"]], writes=[b["esink"]])
            K.op(DVE, lambda e: e.tensor_tensor(out=lamw[:, 0:64], in0=lamw[:, 0:64], in1=lamw[:, 64:128], op=ALU.mult),
                 reads=[b["lamw"]], writes=[b["lamw"]])
            K.op(DVE, lambda e: e.tensor_tensor(out=lamw[:, 128:192], in0=lamw[:, 128:192], in1=lamw[:, 192:256], op=ALU.mult),
                 reads=[b["lamw"]], writes=[b["lamw"]])
            K.op(DVE, lambda e: e.reduce_sum(out=lamt[:, 0:1], in_=lamw[:, 0:64], axis=AX.X), reads=[b["lamw"]], writes=[b["lamt"]])
            K.op(DVE, lambda e: e.reduce_sum(out=lamt[:, 1:2], in_=lamw[:, 128:192], axis=AX.X), reads=[b["lamw"]], writes=[b["lamt"]])
            K.op(ACT, lambda e: e.activation(out=lamt[:, 2:4], in_=lamt[:, 0:2], func=AF.Exp), reads=[b["lamt"]], writes=[b["lamt"]])
            K.op(DVE, lambda e: e.scalar_tensor_tensor(out=neglam[:], in0=lamt[:, 3:4], scalar=-0.2, in1=lamt[:, 2:3],
                                                       op0=ALU.add, op1=ALU.subtract),
                 reads=[b["lamt"]], writes=[b["neglam"]])
            K.op(ACT, lambda e: e.activation(out=cact[:], in_=cact[:], func=AF.Silu), reads=[b["cact"]], writes=[b["cact"]])

            for n in range(12):
                sl = n % 2
                K.dma(SP, adst[sl][:], w_ada[:, n * 512:(n + 1) * 512].rearrange("(k p) n -> p k n", p=128), None,
                      writes=[b_adst[sl]])
                K.dma(SP, bst[sl][0:NSEQ, :], b_ada[:, n * 512:(n + 1) * 512].partition_broadcast(NSEQ), None, writes=[b_bst[sl]])
                pb = n % 2
                for k in range(8):
                    K.op(PE, lambda e, k=k, sl=sl, pb=pb: e.matmul(bank(pb)[0:NSEQ, :], lhsT=cact[:, k, :], rhs=adst[sl][:, k, :],
                                                                   start=(k == 0), stop=(k == 7)),
                         reads=[b["cact"], b_adst[sl]], writes=[PB[pb]], inc=(k == 7))
                K.op(DVE, lambda e, sl=sl, pb=pb: e.tensor_tensor(out=modsb[sl][0:NSEQ, :], in0=bank(pb)[0:NSEQ, :], in1=bst[sl][0:NSEQ, :], op=ALU.add),
                     reads=[PB[pb], b_bst[sl]], writes=[b_modsb[sl]])
                K.dma(SP, mod_scr[:, n * 512:(n + 1) * 512], modsb[sl][0:NSEQ, :], s_mod, reads=[b_modsb[sl]], writes=[b["mod_scr"]])
            K.barrier()
            _ck('pro')

            def rstd_from_ss(ss_ap, scale, sbuf=None):
                sbuf = sbuf if sbuf is not None else b["stat"]
                K.op(ACT, lambda e: e.activation(out=ss_ap, in_=ss_ap, func=AF.Sqrt, scale=scale, bias=EPS),
                     reads=[sbuf], writes=[sbuf])
                K.op(DVE, lambda e: e.reciprocal(out=ss_ap, in_=ss_ap), reads=[sbuf], writes=[sbuf])

            def load_mod_vec(dst, dst_buf, seq, v, sem=None):
                K.dma(SP, dst[:], mod_scr[seq:seq + 1, v * D:(v + 1) * D].partition_broadcast(128), None,
                      reads=[b["mod_scr"]], writes=[dst_buf])

            for seq in range(NSEQ):
                load_mod_vec(B1, b["B1"], seq, 0)
                load_mod_vec(A1, b["A1"], seq, 1)
                K.dma(SP, tmpf[:], g_mix.partition_broadcast(128), None, writes=[b["tmpf"]])
                K.op(DVE, lambda e: e.scalar_tensor_tensor(out=A1[:], in0=A1[:], scalar=1.0, in1=tmpf[:], op0=ALU.add, op1=ALU.mult),
                     reads=[b["A1"], b["tmpf"]], writes=[b["A1"]])

                tmpf_s = [tmpf, tmpfb]
                hb_s = [hb, hbb]

                def s1_stage1(t):
                    sl = t % 3
                    d2 = t % 2
                    K.dma(SP, xs[sl][:], x[seq, t * 128:(t + 1) * 128, :], None, writes=[b_xs[sl]])
                    ssap = stat[:, 8 + d2:9 + d2]
                    K.op(ACT, lambda e: e.activation(out=junk[:], in_=xs[sl][:], func=AF.Square, accum_out=ssap),
                         reads=[b_xs[sl]], writes=[b_st[d2]])
                    rstd_from_ss(ssap, 1.0 / D, b_st[d2])
                    K.op(DVE, lambda e: e.scalar_tensor_tensor(out=tmpf_s[d2][:], in0=xs[sl][:], scalar=ssap, in1=A1[:],
                                                               op0=ALU.mult, op1=ALU.mult),
                         reads=[b_xs[sl], b_st[d2], b["A1"]], writes=[b_tmpf2s[d2]])
                    K.op(POOL, lambda e: e.tensor_tensor(out=hb_s[d2][:], in0=tmpf_s[d2][:], in1=B1[:], op=ALU.add),
                         reads=[b_tmpf2s[d2], b["B1"]], writes=[b_hb2s[d2]])

                def s1_stage2(t):
                    d2 = t % 2
                    pbi = t % 2
                    pbv = bank(pbi).bitcast(BF16)
                    for k in range(8):
                        K.op(PE, lambda e, k=k: e.transpose(out=pbv[:, k * 128:(k + 1) * 128], in_=hb_s[d2][:, k * 128:(k + 1) * 128],
                                                            identity=ident_b[:]),
                             reads=[b_hb2s[d2], b["ident_b"]], writes=[PB[pbi]], inc=(k == 7))
                    K.op(ACT, lambda e: e.copy(out=hT[:, :, t * 128:(t + 1) * 128], in_=pbv.rearrange("p (k c) -> p k c", k=8)),
                         reads=[PB[pbi]], writes=[b["hT"]])

                for t in range(NT + 1):
                    if t < NT:
                        s1_stage1(t)
                    if t >= 1:
                        s1_stage2(t - 1)
                K.barrier()
                _ck('b1')

                K.dma(SP, cosb[:], c_cos, None, writes=[b["cosb"]])
                K.dma(SP, sinb[:], c_sin, None, writes=[b["sinb"]])
                K.op(POOL, lambda e: e.memset(kz[0][:], 0.0), writes=[b["kz0"]])
                K.op(POOL, lambda e: e.memset(kz[1][:], 0.0), writes=[b["kz1"]])

                wcnt = [0]

                def load_wunit(col0, ncols):
                    sl = wcnt[0] % 2
                    wcnt[0] += 1
                    K.dma(SP, wstg[sl][:, :, 0:ncols], w_in[:, col0:col0 + ncols].rearrange("(k p) n -> p k n", p=128), None,
                          writes=[b_wstg[sl]])
                    K.op(POOL, lambda e, sl=sl: e.tensor_copy(out=wub[sl][:, :, 0:ncols], in_=wstg[sl][:, :, 0:ncols]),
                         reads=[b_wstg[sl]], writes=[b_wub[sl]])
                    return sl

                for piece, (c0, ncol) in enumerate([(2048, 256), (2304, 256), (2560, 128)]):
                    sl = load_wunit(c0, ncol)
                    for t in range(NT):
                        pbi = t % 2
                        for k in range(8):
                            K.op(PE, lambda e, k=k, t=t, sl=sl, pbi=pbi, ncol=ncol: e.matmul(
                                bank(pbi)[:, 0:ncol], lhsT=hT2[:, k, t * 128:(t + 1) * 128], rhs=wub[sl][:, k, 0:ncol],
                                start=(k == 0), stop=(k == 7)),
                                reads=[b["hT"], b_wub[sl]], writes=[PB[pbi]], inc=(k == 7))
                        if piece < 2:
                            K.op(ACT, lambda e, t=t, pbi=pbi, piece=piece: e.copy(out=dv[:, t, piece * 256:(piece + 1) * 256],
                                                                                  in_=bank(pbi)[:, 0:256]),
                                 reads=[PB[pbi]], writes=[b["dv"]])
                        else:
                            K.op(ACT, lambda e, pbi=pbi: e.copy(out=svsb[:], in_=bank(pbi)[:, 0:128]), reads=[PB[pbi]], writes=[b["svsb"]])
                            for hk in range(2):
                                for dup in range(2):
                                    K.op(POOL, lambda e, t=t, hk=hk, dup=dup: e.tensor_copy(
                                        out=svd[:, t, hk, dup * 64:(dup + 1) * 64], in_=svsb[:, hk * 64:(hk + 1) * 64]),
                                        reads=[b["svsb"]], writes=[b["svd"]])

                _ck('v')
                sl_next = load_wunit(0, 256)
                for u in range(8):
                    sl = sl_next
                    def rope_a(ridx):
                        j, which = divmod(ridx, 2)
                        cs = slice(j * 512, (j + 1) * 512)
                        pbi = (0, 1, 2, 7)[ridx % 4]
                        rsl = ridx % 3
                        for k in range(8):
                            K.op(PE, lambda e, k=k: e.matmul(
                                bank(pbi), lhsT=wub[sl][:, k, which * 128:(which + 1) * 128], rhs=hT2[:, k, cs],
                                start=(k == 0), stop=(k == 7)),
                                reads=[b["hT"], b_wub[sl]], writes=[PB[pbi]], inc=(k == 7))
                        qraw = qraw_s[rsl]
                        K.op(ACT, lambda e: e.copy(out=qraw[:], in_=bank(pbi)), reads=[PB[pbi]], writes=[b_qraw[rsl], PB[pbi]])

                    def rope_b(ridx):
                        j, which = divmod(ridx, 2)
                        cs = slice(j * 512, (j + 1) * 512)
                        pbi = (0, 1, 2, 7)[ridx % 4]
                        rb = (3, 4, 5, 6)[ridx % 4]
                        rsl = ridx % 3
                        qraw, rt1, rt2 = qraw_s[rsl], rt1_s[rsl], rt2_s[rsl]
                        bq, b1_, b2_ = b_qraw[rsl], b_rt1[rsl], b_rt2[rsl]
                        K.op(PE, lambda e: e.matmul(bank(rb), lhsT=rt_b[:], rhs=qraw[:], start=True, stop=True),
                             reads=[b["rt_b"], bq], writes=[PB[rb]])
                        K.op(DVE, lambda e: e.tensor_tensor(out=rt1[:], in0=bank(pbi), in1=cosb[:, cs], op=ALU.mult),
                             reads=[PB[pbi], b["cosb"]], writes=[b1_, PB[pbi]])
                        K.op(DVE, lambda e: e.tensor_tensor(out=rt2[:], in0=bank(rb), in1=sinb[:, cs], op=ALU.mult),
                             reads=[PB[rb], b["sinb"]], writes=[b2_])
                        if which == 0:
                            K.op(POOL, lambda e: e.tensor_tensor(out=qT[:, cs], in0=rt1[:], in1=rt2[:], op=ALU.add),
                                 reads=[b1_, b2_], writes=[b["qT"]])
                        else:
                            K.op(DVE, lambda e: e.tensor_tensor(out=kz[0][0:64, cs], in0=rt1[0:64, :], in1=rt2[0:64, :], op=ALU.add),
                                 reads=[b1_, b2_], writes=[b["kz0"]])
                            K.op(DVE, lambda e: e.tensor_tensor(out=kz[1][64:128, cs], in0=rt1[64:128, :], in1=rt2[64:128, :], op=ALU.add),
                                 reads=[b1_, b2_], writes=[b["kz1"]])

                    nrope = 2 * NB
                    for ridx in range(nrope + 1):
                        if ridx < nrope:
                            rope_a(ridx)
                        if ridx >= 1:
                            rope_b(ridx - 1)

                    _ck(f'proj{u}')
                    if u < 7:
                        sl_next = load_wunit((u + 1) * 256, 256)
                    if u < 4:
                        h = u
                        for j in range(NB):
                            items = [(c, m) for c in range(4 * j + 4) for m in range(2)]
                            nit = len(items)
                            last_c = 4 * j + 3

                            def s_stage(idx):
                                c, m = items[idx]
                                i = c - 4 * j
                                q0 = max(i, 0) * 128
                                sb_i = (0, 1, 2, 7)[idx % 4]
                                pt_i = idx % 4
                                K.op(PE, lambda e, c=c, m=m, q0=q0, sb_i=sb_i: e.matmul(
                                    bank(sb_i)[:, q0:512], lhsT=kz[m][:, c * 128:(c + 1) * 128], rhs=qT[:, j * 512 + q0:(j + 1) * 512],
                                    start=True, stop=True),
                                    reads=[b["kz0"], b["kz1"], b["qT"]], writes=[PB[sb_i]])
                                K.op(ACT, lambda e, q0=q0, sb_i=sb_i, pt_i=pt_i: e.activation(
                                    out=pT[pt_i][:, q0:512], in_=bank(sb_i)[:, q0:512], func=AF.Exp, scale=0.125),
                                    reads=[PB[sb_i]], writes=[b_pT[pt_i]])
                                if i >= 0:
                                    K.op(DVE, lambda e, q0=q0, pt_i=pt_i: e.tensor_tensor(
                                        out=pT[pt_i][:, q0:q0 + 128], in0=pT[pt_i][:, q0:q0 + 128], in1=tri_b[:], op=ALU.mult),
                                        reads=[b_pT[pt_i], b["tri_b"]], writes=[b_pT[pt_i]])

                            def pv_stage(idx):
                                c, m = items[idx]
                                i = c - 4 * j
                                q0 = max(i, 0) * 128
                                pt_i = idx % 4
                                ob = 3 + m
                                lb = 5 + m
                                K.op(PE, lambda e, c=c, q0=q0, pt_i=pt_i, ob=ob: e.matmul(
                                    bank(ob)[:, q0:512], lhsT=dv[:, c, h * 128:(h + 1) * 128], rhs=pT[pt_i][:, q0:512],
                                    start=(c == 0), stop=(c == last_c)),
                                    reads=[b["dv"], b_pT[pt_i]], writes=[PB[ob]], inc=False)
                                K.op(PE, lambda e, c=c, q0=q0, pt_i=pt_i, lb=lb: e.matmul(
                                    bank(lb)[:, q0:512], lhsT=ones_b[:], rhs=pT[pt_i][:, q0:512],
                                    start=(c == 0), stop=(c == last_c)),
                                    reads=[b["ones_b"], b_pT[pt_i]], writes=[PB[lb]])

                            for idx in range(nit + 3):
                                if idx < nit:
                                    s_stage(idx)
                                if idx >= 3:
                                    pv_stage(idx - 3)
                            cs = slice(j * 512, (j + 1) * 512)
                            K.op(ACT, lambda e: e.activation(out=tA[:], in_=bank(5), func=AF.Ln), reads=[PB[5]], writes=[b["tA"]])
                            K.op(DVE, lambda e: e.tensor_copy(out=tB[:], in_=bank(3)), reads=[PB[3]], writes=[b["tB"]])
                            K.op(ACT, lambda e: e.activation(out=tD[:], in_=bank(6), func=AF.Ln), reads=[PB[6]], writes=[b["tD"]])
                            K.op(DVE, lambda e: e.tensor_copy(out=tC[:], in_=bank(4)), reads=[PB[4]], writes=[b["tC"]])
                            K.op(ACT, lambda e: e.activation(out=tA[:], in_=tA[:], func=AF.Exp, scale=-1.0), reads=[b["tA"]], writes=[b["tA"]])
                            K.op(ACT, lambda e: e.activation(out=tD[:], in_=tD[:], func=AF.Exp, scale=-1.0), reads=[b["tD"]], writes=[b["tD"]])
                            K.op(DVE, lambda e: e.tensor_tensor(out=tB[:], in0=tB[:], in1=tA[:], op=ALU.mult),
                                 reads=[b["tB"], b["tA"]], writes=[b["tB"]])
                            K.op(DVE, lambda e: e.tensor_tensor(out=tC[:], in0=tC[:], in1=tD[:], op=ALU.mult),
                                 reads=[b["tC"], b["tD"]], writes=[b["tC"]])
                            K.op(DVE, lambda e: e.scalar_tensor_tensor(out=tB[:], in0=tC[:], scalar=neglam[:, 0:1], in1=tB[:],
                                                                       op0=ALU.mult, op1=ALU.add),
                                 reads=[b["tC"], b["tB"], b["neglam"]], writes=[b["tB"]])
                            K.op(ACT, lambda e: e.activation(out=sqb[:], in_=tB[:], func=AF.Square), reads=[b["tB"]], writes=[b["sqb"]])
                            K.op(PE, lambda e: e.matmul(bank(7), lhsT=ones_b[:], rhs=sqb[:], start=True, stop=True),
                                 reads=[b["ones_b"], b["sqb"]], writes=[PB[7]])
                            K.op(ACT, lambda e: e.activation(out=tA[:], in_=bank(7), func=AF.Ln, scale=1.0 / 128, bias=epsc[:, 0:1]),
                                 reads=[PB[7], b["epsc"]], writes=[b["tA"]])
                            K.op(ACT, lambda e: e.activation(out=tA[:], in_=tA[:], func=AF.Exp, scale=-0.5), reads=[b["tA"]], writes=[b["tA"]])
                            K.op(DVE, lambda e, cs=cs: e.scalar_tensor_tensor(out=oT[:, h, cs], in0=tB[:], scalar=gds[:, 0:1], in1=tA[:],
                                                                              op0=ALU.mult, op1=ALU.mult),
                                 reads=[b["tB"], b["gds"], b["tA"]], writes=[b_oT[h]])
                    else:
                        ch = u - 4
                        hk = ch // 2
                        for j in range(NB):
                            items = list(range(4 * j, 4 * j + 4))
                            nit = len(items)

                            def s_stage(idx):
                                t = items[idx]
                                sb_i = (0, 1, 2, 7)[idx % 4]
                                pt_i = idx % 4
                                lo = 0 if t > 0 else 128
                                for s in range(2):
                                    c0 = s * 256
                                    if t > 0:
                                        K.op(PE, lambda e, c0=c0, s=s: e.matmul(
                                            bank(sb_i)[:, c0:c0 + 128], lhsT=kz[s][:, (t - 1) * 128:t * 128], rhs=qT[:, t * 128:(t + 1) * 128],
                                            start=True, stop=True),
                                            reads=[b["kz0"], b["kz1"], b["qT"]], writes=[PB[sb_i]], inc=False)
                                    K.op(PE, lambda e, c0=c0, s=s: e.matmul(
                                        bank(sb_i)[:, c0 + 128:c0 + 256], lhsT=kz[s][:, t * 128:(t + 1) * 128], rhs=qT[:, t * 128:(t + 1) * 128],
                                        start=True, stop=True),
                                        reads=[b["kz0"], b["kz1"], b["qT"]], writes=[PB[sb_i]], inc=(s == 1))
                                src3 = bank(sb_i).rearrange("p (s c) -> p s c", s=2)[:, :, lo:256]
                                dst3 = pT[pt_i][:].rearrange("p (s c) -> p s c", s=2)[:, :, lo:256]
                                msk3 = swam_b[:, lo:256].unsqueeze(1).broadcast_to([128, 2, 256 - lo])
                                K.op(ACT, lambda e: e.activation(out=dst3, in_=src3, func=AF.Exp, scale=0.125),
                                     reads=[PB[sb_i]], writes=[b_pT[pt_i]])
                                K.op(DVE, lambda e: e.tensor_tensor(out=dst3, in0=dst3, in1=msk3, op=ALU.mult),
                                     reads=[b_pT[pt_i], b["swam_b"]], writes=[b_pT[pt_i]])

                            def pv_stage(idx):
                                t = items[idx]
                                pt_i = idx % 4
                                tq = t - 4 * j
                                reg = slice(tq * 128, (tq + 1) * 128)
                                for s in range(2):
                                    c0 = s * 256
                                    ob = 3 + s
                                    lb = 5 + s
                                    first = True
                                    if t > 0:
                                        K.op(PE, lambda e, c0=c0, ob=ob: e.matmul(
                                            bank(ob)[:, reg], lhsT=svd[:, t - 1, hk, :], rhs=pT[pt_i][:, c0:c0 + 128], start=True, stop=False),
                                            reads=[b["svd"], b_pT[pt_i]], writes=[PB[ob]], inc=False)
                                        first = False
                                    K.op(PE, lambda e, c0=c0, ob=ob, first=first: e.matmul(
                                        bank(ob)[:, reg], lhsT=svd[:, t, hk, :], rhs=pT[pt_i][:, c0 + 128:c0 + 256], start=first, stop=True),
                                        reads=[b["svd"], b_pT[pt_i]], writes=[PB[ob]], inc=False)
                                    if t > 0:
                                        K.op(PE, lambda e, c0=c0, lb=lb: e.matmul(
                                            bank(lb)[:, reg], lhsT=ones_b[:], rhs=pT[pt_i][:, c0:c0 + 128], start=True, stop=False),
                                            reads=[b["ones_b"], b_pT[pt_i]], writes=[PB[lb]], inc=False)
                                    K.op(PE, lambda e, c0=c0, lb=lb, first=first: e.matmul(
                                        bank(lb)[:, reg], lhsT=ones_b[:], rhs=pT[pt_i][:, c0 + 128:c0 + 256], start=first, stop=True),
                                        reads=[b["ones_b"], b_pT[pt_i]], writes=[PB[lb]], inc=(s == 1))

                            for idx in range(nit + 3):
                                if idx < nit:
                                    s_stage(idx)
                                if idx >= 3:
                                    pv_stage(idx - 3)
                            cs = slice(j * 512, (j + 1) * 512)
                            for s in range(2):
                                pr = slice(s * 64, (s + 1) * 64)
                                K.op(DVE, lambda e, s=s, pr=pr: e.tensor_scalar(out=tA[pr, :], in0=bank(5 + s)[pr, :], scalar1=esink[pr, ch:ch + 1],
                                                                                scalar2=None, op0=ALU.add),
                                     reads=[PB[5 + s], b["esink"]], writes=[b["tA"]])
                                K.op(ACT, lambda e, pr=pr: e.activation(out=tA[pr, :], in_=tA[pr, :], func=AF.Ln), reads=[b["tA"]], writes=[b["tA"]])
                                K.op(ACT, lambda e, pr=pr: e.activation(out=tA[pr, :], in_=tA[pr, :], func=AF.Exp, scale=-1.0), reads=[b["tA"]], writes=[b["tA"]])
                                K.op(DVE, lambda e, s=s, pr=pr, cs=cs: e.tensor_tensor(out=oT[pr, 4 + ch, cs], in0=bank(3 + s)[pr, :], in1=tA[pr, :],
                                                                                       op=ALU.mult),
                                     reads=[PB[3 + s], b["tA"]], writes=[b_oT[4 + ch]])
                    _ck(f'unit{u}')
                K.barrier()
                _ck('b2')

                load_mod_vec(g1t, b["g1t"], seq, 2)
                for i in range(4):
                    sl = i % 2
                    K.dma(SP, wostg[sl][:], w_out[i * 256:(i + 1) * 256, :].rearrange("(k p) n -> p k n", p=128), None,
                          writes=[b_wostg[sl]])
                    for kk in range(2):
                        K.op(DVE, lambda e, i=i, kk=kk, sl=sl: e.tensor_tensor(out=wob[:, 2 * i + kk, :], in0=wostg[sl][:, kk, :], in1=g1t[:],
                                                                               op=ALU.mult),
                             reads=[b_wostg[sl], b["g1t"]], writes=[b["wob"]])
                for t in range(NT):
                    sl = t % 2
                    K.dma(SP, xs5[sl][:], x[seq, t * 128:(t + 1) * 128, :], None, writes=[b_xs5[sl]])
                    pp = t % 2
                    for half in range(2):
                        for kk in range(8):
                            K.op(PE, lambda e, t=t, kk=kk, half=half, pp=pp: e.matmul(
                                bank(2 * pp + half), lhsT=oT[:, kk, t * 128:(t + 1) * 128], rhs=wob[:, kk, half * 512:(half + 1) * 512],
                                start=(kk == 0), stop=(kk == 7)),
                                reads=[b_oT[kk], b["wob"]], writes=[PB[2 * pp + half]], inc=(kk == 7))
                    K.op(DVE, lambda e, t=t, sl=sl, pp=pp: e.tensor_tensor(out=acc[:, t, :], in0=ps[pp][:], in1=xs5[sl][:], op=ALU.add),
                         reads=[PB[2 * pp], PB[2 * pp + 1], b_xs5[sl]], writes=[b_acc[t]])
                K.barrier()
                _ck('b3')

                load_mod_vec(B2, b["B2"], seq, 3)
                load_mod_vec(A2, b["A2"], seq, 4)
                load_mod_vec(G2, b["G2"], seq, 5)
                K.dma(SP, tmpf2[:], g_ffn.partition_broadcast(128), None, writes=[b["tmpf2"]])
                K.op(DVE, lambda e: e.scalar_tensor_tensor(out=A2[:], in0=A2[:], scalar=1.0, in1=tmpf2[:], op0=ALU.add, op1=ALU.mult),
                     reads=[b["A2"], b["tmpf2"]], writes=[b["A2"]])

                for p in range(NPASS):
                    tmpX = [tmpf2, ytmp[0]]
                    b_tmpX = [b["tmpf2"], b_ytmp[0]]
                    h2X = [h2f, ytmp[1]]
                    b_h2X = [b["h2f"], b_ytmp[1]]

                    def s6_st1(tt):
                        t = p * NTP + tt
                        d2 = tt % 2
                        ssap = stat[:, 10 + d2:11 + d2]
                        K.op(ACT, lambda e: e.activation(out=junk2[:], in_=acc[:, t, :], func=AF.Square, accum_out=ssap),
                             reads=[b_acc[t]], writes=[b_st[2 + d2]])
                        rstd_from_ss(ssap, 1.0 / D, b_st[2 + d2])
                        K.op(DVE, lambda e: e.scalar_tensor_tensor(out=tmpX[d2][:], in0=acc[:, t, :], scalar=ssap, in1=A2[:],
                                                                   op0=ALU.mult, op1=ALU.mult),
                             reads=[b_acc[t], b_st[2 + d2], b["A2"]], writes=[b_tmpX[d2]])
                        K.op(POOL, lambda e: e.tensor_tensor(out=h2X[d2][:], in0=tmpX[d2][:], in1=B2[:], op=ALU.add),
                             reads=[b_tmpX[d2], b["B2"]], writes=[b_h2X[d2]])

                    def s6_st2(tt):
                        d2 = tt % 2
                        pp = tt % 2
                        fs = tt % 2
                        for k in range(8):
                            K.op(PE, lambda e, k=k: e.transpose(out=ps[pp][:, k * 128:(k + 1) * 128], in_=h2X[d2][:, k * 128:(k + 1) * 128],
                                                                identity=ident_f[:]),
                                 reads=[b_h2X[d2], b["ident_f"]], writes=[PB[2 * pp], PB[2 * pp + 1]], inc=(k == 7))
                        K.op(DVE, lambda e: e.tensor_copy(out=h2Tf[fs][:], in_=ps[pp][:].rearrange("p (k c) -> p k c", k=8)),
                             reads=[PB[2 * pp], PB[2 * pp + 1]], writes=[b_h2Tf[fs]])
                        K.op(ACT, lambda e: e.copy(out=h2T[:, :, tt * 128:(tt + 1) * 128], in_=h2Tf[fs][:]),
                             reads=[b_h2Tf[fs]], writes=[b["h2T"]])

                    def s6_st3(tt):
                        fs = tt % 2
                        lb = 4 + tt % 2
                        for k in range(8):
                            K.op(PE, lambda e, k=k: e.matmul(bank(lb)[:, 0:20], lhsT=h2Tf[fs][:, k, :], rhs=wr_f[:, k, :],
                                                             start=(k == 0), stop=(k == 7)),
                                 reads=[b_h2Tf[fs], b["wr_f"]], writes=[PB[lb]], inc=(k == 7))
                        K.op(DVE, lambda e: e.tensor_tensor(out=lg[:, tt, :], in0=bank(lb)[:, 0:20], in1=br_f[:], op=ALU.add),
                             reads=[PB[lb], b["br_f"]], writes=[b["lg"]])

                    for i in range(NTP + 2):
                        if i < NTP:
                            s6_st1(i)
                        if 1 <= i <= NTP:
                            s6_st2(i - 1)
                        if i >= 2:
                            s6_st3(i - 2)

                    RW, RS = [b["rw"]], [b["rs"]]

                    def bc(ap2):
                        return ap2.unsqueeze(2).broadcast_to([128, NTP, 4])

                    def vop(fn, reads, writes):
                        K.op(DVE, fn, reads=reads, writes=writes)

                    LGg = lg[:, :, 0:4]
                    gmax, gsum, gp, m1, m2, dd, ee, w1, w2 = [rs[:, i, :] for i in range(9)]
                    goh, gsh, ig, tmp4, oh1, ig2, oh2, within, gw = [rw[:, i, :, :] for i in range(9)]
                    vop(lambda e: e.tensor_reduce(out=gmax, in_=LGg, axis=AX.X, op=ALU.max), [b["lg"]], RS)
                    vop(lambda e: e.tensor_tensor(out=goh, in0=LGg, in1=bc(gmax), op=ALU.is_equal), [b["lg"], b["rs"]], RW)
                    vop(lambda e: e.tensor_tensor(out=gsh, in0=LGg, in1=bc(gmax), op=ALU.subtract), [b["lg"], b["rs"]], RW)
                    K.op(ACT, lambda e: e.activation(out=gsh, in_=gsh, func=AF.Exp), reads=RW, writes=RW)
                    vop(lambda e: e.tensor_reduce(out=gsum, in_=gsh, axis=AX.X, op=ALU.add), RW, RS)
                    vop(lambda e: e.reciprocal(out=gp, in_=gsum), RS, RS)
                    for g in range(4):
                        Lg = lg[:, :, 4 + 4 * g:8 + 4 * g]
                        gsel = rw[:, 0, :, g:g + 1].broadcast_to([128, NTP, 4])
                        if g == 0:
                            vop(lambda e, Lg=Lg, gsel=gsel: e.tensor_tensor(out=ig, in0=Lg, in1=gsel, op=ALU.mult), [b["lg"], b["rw"]], RW)
                        else:
                            vop(lambda e, Lg=Lg, gsel=gsel: e.tensor_tensor(out=tmp4, in0=Lg, in1=gsel, op=ALU.mult), [b["lg"], b["rw"]], RW)
                            vop(lambda e: e.tensor_tensor(out=ig, in0=ig, in1=tmp4, op=ALU.add), RW, RW)
                    vop(lambda e: e.tensor_reduce(out=m1, in_=ig, axis=AX.X, op=ALU.max), RW, RS)
                    vop(lambda e: e.tensor_tensor(out=oh1, in0=ig, in1=bc(m1), op=ALU.is_equal), RW + RS, RW)
                    vop(lambda e: e.scalar_tensor_tensor(out=ig2, in0=oh1, scalar=-1e30, in1=ig, op0=ALU.mult, op1=ALU.add), RW, RW)
                    vop(lambda e: e.tensor_reduce(out=m2, in_=ig2, axis=AX.X, op=ALU.max), RW, RS)
                    vop(lambda e: e.tensor_tensor(out=oh2, in0=ig2, in1=bc(m2), op=ALU.is_equal), RW + RS, RW)
                    vop(lambda e: e.tensor_tensor(out=dd, in0=m2, in1=m1, op=ALU.subtract), RS, RS)
                    K.op(ACT, lambda e: e.activation(out=ee, in_=dd, func=AF.Exp), reads=RS, writes=RS)
                    vop(lambda e: e.tensor_scalar(out=dd, in0=ee, scalar1=1.0, scalar2=None, op0=ALU.add), RS, RS)
                    vop(lambda e: e.reciprocal(out=w1, in_=dd), RS, RS)
                    vop(lambda e: e.tensor_tensor(out=w2, in0=ee, in1=w1, op=ALU.mult), RS, RS)
                    vop(lambda e: e.tensor_tensor(out=within, in0=oh1, in1=bc(w1), op=ALU.mult), RW + RS, RW)
                    vop(lambda e: e.tensor_tensor(out=tmp4, in0=oh2, in1=bc(w2), op=ALU.mult), RW + RS, RW)
                    vop(lambda e: e.tensor_tensor(out=within, in0=within, in1=tmp4, op=ALU.add), RW, RW)
                    vop(lambda e: e.tensor_tensor(out=gw, in0=goh, in1=bc(gp), op=ALU.mult), RW + RS, RW)
                    for g in range(4):
                        gwb = rw[:, 8, :, g:g + 1].broadcast_to([128, NTP, 4])
                        vop(lambda e, g=g, gwb=gwb: e.tensor_tensor(out=comb[:, :, 4 * g:4 * g + 4], in0=within, in1=gwb, op=ALU.mult),
                            RW, [b["comb"]])

                    first_pass = (seq == 0 and p == 0)
                    ecnt = [0]

                    def load_expert(ex):
                        sl = ex % 2
                        if not first_pass:
                            K.dma(SP, wgb[sl][:].rearrange("p k n -> p (k n)"), wsc_g[ex], None, reads=[b_wsc[ex]], writes=[b_wgb[sl]])
                            K.dma(SP, wubx[sl][:].rearrange("p k n -> p (k n)"), wsc_u[ex], None, reads=[b_wsc[ex]], writes=[b_wubx[sl]])
                            K.dma(SP, wdb[sl][:].rearrange("p k n -> p (k n)"), wsc_d[ex], None, reads=[b_wsc[ex]], writes=[b_wdb[sl]])
                            return
                        stg_t = [estg[0], estg[1], h2f, tmpf2, h2Tf[0], h2Tf[1]]
                        stg_b = [b_estg[0], b_estg[1], b["h2f"], b["tmpf2"], b_h2Tf[0], b_h2Tf[1]]

                        def stg_view(es, k):
                            tt_ = stg_t[es]
                            if es >= 4:
                                return tt_[:].rearrange("p k c -> p (k c)").rearrange("p (k n) -> p k n", k=k)
                            return tt_[:].rearrange("p (k n) -> p k n", k=k)

                        for (src, dstt, dbuf, ce) in ((w_gate, wgb, b_wgb, ACT), (w_up, wubx, b_wubx, POOL)):
                            for i in range(4):
                                es = ecnt[0] % 6
                                ecnt[0] += 1
                                K.dma(SP, stg_view(es, 2),
                                      src[ex, i * 256:(i + 1) * 256, :].rearrange("(k p) n -> p k n", p=128), None, writes=[stg_b[es]])
                                if ce is ACT:
                                    K.op(ce, lambda e, es=es, i=i, dstt=dstt, sl=sl: e.copy(
                                        out=dstt[sl][:, 2 * i:2 * i + 2, :], in_=stg_view(es, 2)),
                                        reads=[stg_b[es]], writes=[dbuf[sl]])
                                else:
                                    K.op(ce, lambda e, es=es, i=i, dstt=dstt, sl=sl: e.tensor_copy(
                                        out=dstt[sl][:, 2 * i:2 * i + 2, :], in_=stg_view(es, 2)),
                                        reads=[stg_b[es]], writes=[dbuf[sl]])
                        for i in range(4):
                            es = ecnt[0] % 6
                            ecnt[0] += 1
                            K.dma(SP, stg_view(es, 1), w_down[ex:ex + 1, i * 128:(i + 1) * 128, :].rearrange("o p n -> p o n"), None, writes=[stg_b[es]])
                            K.op(DVE, lambda e, es=es, i=i, sl=sl: e.tensor_copy(out=wdb[sl][:, i:i + 1, :], in_=stg_view(es, 1)),
                                 reads=[stg_b[es]], writes=[b_wdb[sl]])
                        K.dma(SP, wsc_g[ex], wgb[sl][:].rearrange("p k n -> p (k n)"), None, reads=[b_wgb[sl]], writes=[b_wsc[ex]])
                        K.dma(SP, wsc_u[ex], wubx[sl][:].rearrange("p k n -> p (k n)"), None, reads=[b_wubx[sl]], writes=[b_wsc[ex]])
                        K.dma(SP, wsc_d[ex], wdb[sl][:].rearrange("p k n -> p (k n)"), None, reads=[b_wdb[sl]], writes=[b_wsc[ex]])

                    blocks = [(ex, jb) for ex in range(NE) for jb in range(NBP)]
                    gcnt = [0]
                    ycnt = [0]

                    def stage_a(bi):
                        ex, jb = blocks[bi]
                        sl = ex % 2
                        cs = slice(jb * 512, (jb + 1) * 512)
                        for mch in range(4):
                            gi = gcnt[0] % 2
                            gcnt[0] += 1
                            gb, ub = gi, 2 + gi
                            ai = (bi % 2) * 4 + mch
                            for k in range(8):
                                K.op(PE, lambda e, k=k, sl=sl, mch=mch, gb=gb, cs=cs: e.matmul(
                                    bank(gb), lhsT=wgb[sl][:, k, mch * 128:(mch + 1) * 128], rhs=h2T[:, k, cs],
                                    start=(k == 0), stop=(k == 7)),
                                    reads=[b_wgb[sl], b["h2T"]], writes=[PB[gb]], inc=(k == 7))
                            for k in range(8):
                                K.op(PE, lambda e, k=k, sl=sl, mch=mch, ub=ub, cs=cs: e.matmul(
                                    bank(ub), lhsT=wubx[sl][:, k, mch * 128:(mch + 1) * 128], rhs=h2T[:, k, cs],
                                    start=(k == 0), stop=(k == 7)),
                                    reads=[b_wubx[sl], b["h2T"]], writes=[PB[ub]], inc=(k == 7))
                            K.op(ACT, lambda e, gi=gi, gb=gb: e.activation(out=sg[gi][:], in_=bank(gb), func=AF.Silu),
                                 reads=[PB[gb]], writes=[b_sg[gi]])
                            K.op(DVE, lambda e, gi=gi, ub=ub, ai=ai: e.tensor_tensor(out=aT[ai][:], in0=bank(ub), in1=sg[gi][:], op=ALU.mult),
                                 reads=[PB[ub], b_sg[gi]], writes=[b_aT[ai]])

                    def stage_b(bi):
                        ex, jb = blocks[bi]
                        sl = ex % 2
                        for t4 in range(4):
                            tt = jb * 4 + t4
                            t = p * NTP + tt
                            yi = ycnt[0] % 2
                            yp = 2 + yi
                            ycnt[0] += 1
                            for half in range(2):
                                for mch in range(4):
                                    ai = (bi % 2) * 4 + mch
                                    K.op(PE, lambda e, t4=t4, half=half, mch=mch, ai=ai, yp=yp, sl=sl: e.matmul(
                                        bank(2 * yp + half), lhsT=aT[ai][:, t4 * 128:(t4 + 1) * 128],
                                        rhs=wdb[sl][:, mch, half * 512:(half + 1) * 512], start=(mch == 0), stop=(mch == 3)),
                                        reads=[b_aT[ai], b_wdb[sl]], writes=[PB[2 * yp + half]], inc=(mch == 3))
                            K.op(DVE, lambda e, tt=tt, ex=ex, yp=yp, yi=yi: e.scalar_tensor_tensor(
                                out=ytmp[yi][:], in0=ps[yp][:], scalar=comb[:, tt, ex:ex + 1], in1=G2[:],
                                op0=ALU.mult, op1=ALU.mult),
                                reads=[PB[2 * yp], PB[2 * yp + 1], b["comb"], b["G2"]], writes=[b_ytmp[yi]])
                            K.op(POOL, lambda e, t=t, yi=yi: e.tensor_tensor(out=acc[:, t, :], in0=acc[:, t, :], in1=ytmp[yi][:], op=ALU.add),
                                 reads=[b_acc[t], b_ytmp[yi]], writes=[b_acc[t]])
                        if jb == NBP - 1 and ex + 2 < NE:
                            load_expert(ex + 2)

                    load_expert(0)
                    load_expert(1)
                    for bi in range(len(blocks) + 1):
                        if bi < len(blocks):
                            stage_a(bi)
                        if bi >= 1:
                            stage_b(bi - 1)

                    for tt in range(NTP):
                        t = p * NTP + tt
                        d4 = tt % 4
                        ssap = stat[:, 12 + d4:13 + d4]
                        K.op(ACT, lambda e: e.activation(out=junk2[:], in_=acc[:, t, :], func=AF.Square, accum_out=ssap),
                             reads=[b_acc[t]], writes=[b_st[4 + d4]])
                        rstd_from_ss(ssap, 1.0 / D, b_st[4 + d4])
                        K.op(DVE, lambda e: e.scalar_tensor_tensor(out=acc[:, t, :], in0=acc[:, t, :], scalar=ssap, in1=gfin_bc[:],
                                                                   op0=ALU.mult, op1=ALU.mult),
                             reads=[b_acc[t], b_st[4 + d4], b["gfin_bc"]], writes=[b_acc[t]])
                        K.dma(SP, out[seq, t * 128:(t + 1) * 128, :], acc[:, t, :], s_out, reads=[b_acc[t]])
                K.barrier(new_sems=(seq + 1 < NSEQ))


        except _Stop:
            K.barrier()
            if STOP == 'b2':
                dflat = out[0].rearrange("(p a) d -> p (a d)", p=128)
                for k in range(8):
                    for jj in range(S // 512):
                        K.op(DVE, lambda e, k=k, jj=jj: e.tensor_copy(out=tA[:], in_=oT[:, k, jj * 512:(jj + 1) * 512]),
                             reads=[b_oT[k]], writes=[b["tA"]])
                        K.dma(SP, dflat[:, k * S + jj * 512:k * S + (jj + 1) * 512], tA[:], s_out, reads=[b["tA"]])
            if STOP == 'b3':
                for t in range(NT):
                    K.dma(SP, out[0, t * 128:(t + 1) * 128, :], acc[:, t, :], s_out, reads=[b_acc[t]])
        K.finish([(SP, s_out)])
    return nc


def _consts(S):
    ident = np.eye(128, dtype=np.float32)
    rt = np.zeros((128, 128), dtype=np.float32)
    for p in range(128):
        if p % 64 < 32:
            rt[p + 32, p] = -1.0
        else:
            rt[p - 32, p] = 1.0
    kk = np.arange(128)[:, None]
    qq = np.arange(128)[None, :]
    tri = (kk <= qq).astype(np.float32)
    prev = (qq < kk).astype(np.float32)
    swam = np.concatenate([prev, tri], axis=1)
    inv = (10000.0 ** (-np.arange(0, 64, 2, dtype=np.float32) / np.float32(64))).astype(np.float32)
    ang = np.arange(S, dtype=np.float32)[:, None] * inv[None, :]
    ang = np.concatenate([ang, ang], axis=-1)
    cos = np.cos(ang).astype(np.float32).T
    sin = np.sin(ang).astype(np.float32).T
    cosT = np.concatenate([cos, cos], axis=0).astype(ml_dtypes.bfloat16)
    sinT = np.concatenate([sin, sin], axis=0).astype(ml_dtypes.bfloat16)
    return ident, rt, tri, swam, np.ascontiguousarray(cosT), np.ascontiguousarray(sinT)


_NC_CACHE = {}


def kernel(x, c, w_ada, b_ada, g_mix, w_in, diff_lambda, g_diff_sub, swa_sinks, w_out,
           g_ffn, w_route_group, b_route_group, w_route_expert, b_route_expert,
           w_gate, w_up, w_down, g_final):
    x = np.asarray(x, dtype=np.float32)
    B, S, _ = x.shape
    NSEQ = B // NCORES
    f = lambda a: np.ascontiguousarray(np.asarray(a, dtype=np.float32))
    w_in0 = f(w_in)[0]
    cols = []
    for h in range(4):
        cols.append(w_in0[:, h * 128:(h + 1) * 128])
        cols.append(w_in0[:, 512 + h * 128:512 + (h + 1) * 128])
    for ch in range(4):
        hk = ch // 2
        cols.append(w_in0[:, 1536 + ch * 128:1536 + (ch + 1) * 128])
        skh = w_in0[:, 2048 + hk * 64:2048 + (hk + 1) * 64]
        cols.append(skh)
        cols.append(skh)
    cols.append(w_in0[:, 1024:1536])
    cols.append(w_in0[:, 2176:2304])
    w_in_ext = np.ascontiguousarray(np.concatenate(cols, axis=1))
    assert w_in_ext.shape == (1024, 2688)
    sinks = f(swa_sinks)[0]
    sink_c = np.zeros((128, 4), np.float32)
    for ch in range(4):
        sink_c[0:64, ch] = sinks[2 * ch]
        sink_c[64:128, ch] = sinks[2 * ch + 1]
    ident, rt, tri, swam, cosT, sinT = _consts(S)
    w_r = np.ascontiguousarray(np.concatenate([f(w_route_group)[0], f(w_route_expert)[0]], axis=1))
    b_r = np.ascontiguousarray(np.concatenate([f(b_route_group)[0], f(b_route_expert)[0]])[None, :])
    shared = dict(
        w_ada=f(w_ada)[0], b_ada=f(b_ada), g_mix=f(g_mix), g_ffn=f(g_ffn), g_fin=f(g_final)[None, :],
        w_in=w_in_ext, dlam=f(diff_lambda).reshape(1, 256), gds_c=f(g_diff_sub).reshape(128, 1), sink_c=sink_c,
        w_out=f(w_out)[0], w_r=w_r, b_r=b_r, w_gate=f(w_gate)[0], w_up=f(w_up)[0], w_down=f(w_down)[0],
        c_ident=ident, c_rt=rt, c_tri=tri, c_swam=swam, c_cos=cosT, c_sin=sinT,
    )
    cf = f(c)
    in_maps = []
    for i in range(NCORES):
        m = dict(shared)
        m["x"] = np.ascontiguousarray(x[i * NSEQ:(i + 1) * NSEQ])
        cc = cf[i * NSEQ:(i + 1) * NSEQ]
        m["cT"] = np.ascontiguousarray(cc.reshape(NSEQ, 8, 128).transpose(2, 1, 0))
        in_maps.append(m)
    key = (NSEQ, S)
    if key not in _NC_CACHE:
        _NC_CACHE[key] = build_nc(NSEQ, S)
    nc = _NC_CACHE[key]
    res = run_bass_kernel_spmd(nc, in_maps, core_ids=list(range(NCORES)))
    outs = [np.asarray(r["out"], dtype=np.float32).reshape(NSEQ, S, D) for r in res.results]
    return np.concatenate(outs, axis=0)
```

```python
import math
import types
from contextlib import ExitStack

import numpy as np
import ml_dtypes

import concourse.bass as bass
import concourse.mybir as mybir
from concourse.bass_utils import run_bass_kernel_spmd

F32 = mybir.dt.float32
BF16 = mybir.dt.bfloat16
AF = mybir.ActivationFunctionType
ALU = mybir.AluOpType
AX = mybir.AxisListType

D = 1024
NCORES = 8
EPS = 1e-6
SB_BASE = 16512
SB_LIMIT = 229376
KB = 1024


def _freeze(fn):
    if fn.__closure__ is None:
        return fn
    cells = []
    for c in fn.__closure__:
        try:
            cells.append(types.CellType(c.cell_contents))
        except ValueError:
            cells.append(c)
    return types.FunctionType(fn.__code__, fn.__globals__, fn.__name__, fn.__defaults__, tuple(cells))


class Sem:
    def __init__(self, h):
        self.h = h
        self.v = 0


class Eng:
    def __init__(self, name, sem, is_pe=False):
        self.name = name
        self.sem = sem
        self.seen = {}
        self.q = []
        self.is_pe = is_pe


class Buf:
    __slots__ = ("name", "w", "r", "dsem")

    def __init__(self, name, reg):
        self.name = name
        self.w = {}
        self.r = {}
        self.dsem = None
        reg.append(self)


class Sched:
    def __init__(self, nc, stack):
        self.nc = nc
        self.stack = stack
        self.bufs = []
        self.dma_sems = []
        self.nsem = 0
        self.pe = Eng("pe", self.new_sem("pe"), is_pe=True)
        self.act = Eng("act", self.new_sem("act"))
        self.dve = Eng("dve", self.new_sem("dve"))
        self.pool = Eng("pool", self.new_sem("pool"))
        self.sp = Eng("sp", self.new_sem("sp"))
        self.engs = [self.pe, self.act, self.dve, self.pool, self.sp]

    def new_sem(self, name):
        self.nsem += 1
        h = self.stack.enter_context(self.nc.semaphore(f"s{self.nsem}_{name}"))
        return Sem(h)

    def new_dma_sem(self, name):
        s = self.new_sem(name)
        self.dma_sems.append(s)
        return s

    def buf(self, name):
        return Buf(name, self.bufs)

    def _waits(self, E, deps):
        for s, v in deps.items():
            if s is E.sem:
                if E.is_pe or v > s.v:
                    continue
            if E.seen.get(s, 0) >= v:
                continue
            assert v <= s.v, f"wait on pending count {E.name} {v} > {s.v}"
            E.q.append(lambda eng, h=s.h, v=v: eng.wait_ge(h, v))
            E.seen[s] = v

    @staticmethod
    def _deps(reads, writes):
        deps = {}
        for b in reads:
            for s, v in b.w.items():
                if v > deps.get(s, 0):
                    deps[s] = v
        for b in writes:
            for s, v in b.w.items():
                if v > deps.get(s, 0):
                    deps[s] = v
            for s, v in b.r.items():
                if v > deps.get(s, 0):
                    deps[s] = v
        return deps

    def op(self, E, fn, reads=(), writes=(), inc=True):
        fn = _freeze(fn)
        self._waits(E, self._deps(reads, writes))
        if inc:
            E.sem.v += 1
            val = E.sem.v
            E.q.append(lambda eng, fn=fn, h=E.sem.h: fn(eng).then_inc(h, 1))
        else:
            val = E.sem.v + 1
            E.q.append(lambda eng, fn=fn: fn(eng))
        s = E.sem
        for b in reads:
            if b.r.get(s, 0) < val:
                b.r[s] = val
        for b in writes:
            if b.w.get(s, 0) < val:
                b.w[s] = val

    def dma(self, Q, out, in_, sem, reads=(), writes=()):
        if sem is None:
            wb = writes[0]
            if wb.dsem is None:
                wb.dsem = self.new_dma_sem("d_" + wb.name)
            sem = wb.dsem
        self._waits(Q, self._deps(reads, writes))
        sem.v += 16
        Q.q.append(lambda eng, out=out, in_=in_, h=sem.h: eng.dma_start(out=out, in_=in_).then_inc(h, 16))
        for b in reads:
            b.r[sem] = sem.v
        for b in writes:
            b.w[sem] = sem.v

    def barrier(self, new_sems=False):
        allsems = [E.sem for E in self.engs[:4]] + self.dma_sems
        for E in self.engs:
            deps = {s: s.v for s in allsems if s.v > 0 and s is not E.sem}
            self._waits(E, deps)
        for b in self.bufs:
            b.w.clear()
            b.r.clear()
        if new_sems:
            for E in self.engs[:4]:
                E.sem = self.new_sem(E.name)

    def finish(self, final_waits):
        for E, s in final_waits:
            self._waits(E, {s: s.v})
        with self.nc.Block() as block:
            @block.tensor
            def _(e):
                for f in self.pe.q:
                    f(e)

            @block.scalar
            def _(e):
                for f in self.act.q:
                    f(e)

            @block.vector
            def _(e):
                for f in self.dve.q:
                    f(e)

            @block.gpsimd
            def _(e):
                for f in self.pool.q:
                    f(e)

            @block.sync
            def _(e):
                for f in self.sp.q:
                    f(e)


class Region:
    def __init__(self, nc, start, end, tag):
        self.nc = nc
        self.off = start
        self.end = end
        self.tag = tag

    def alloc(self, name, shape, dt):
        esz = 4 if dt == F32 else 2
        nbytes = int(np.prod(shape[1:])) * esz
        o = self.off
        self.off += (nbytes + 63) // 64 * 64
        assert self.off <= self.end, f"SBUF region {self.tag} overflow at {name}: {self.off} > {self.end}"
        return self.nc.alloc_sbuf_tensor_at(f"{self.tag}_{name}", shape, dt, offset=o)


class _Stop(Exception):
    pass


STOP = None


def build_nc(NSEQ, S):
    NT = S // 128
    NB = S // 512
    TP = min(S, 1024)
    NPASS = S // TP
    NTP = TP // 128
    NBP = TP // 512
    NE = 16

    nc = bass.Bass("TRN2", target_bir_lowering=False)

    def din(name, shape, dt=F32):
        return nc.dram_tensor(name, shape, dt, kind="ExternalInput").ap()

    x = din("x", [NSEQ, S, D])
    cT = din("cT", [128, 8, NSEQ])
    w_ada = din("w_ada", [D, 6 * D])
    b_ada = din("b_ada", [1, 6 * D])
    g_mix = din("g_mix", [1, D])
    g_ffn = din("g_ffn", [1, D])
    g_fin = din("g_fin", [1, D])
    w_in = din("w_in", [D, 2688])
    dlam = din("dlam", [1, 256])
    gds_c = din("gds_c", [128, 1])
    sink_c = din("sink_c", [128, 4])
    w_out = din("w_out", [D, D])
    w_r = din("w_r", [D, 20])
    b_r = din("b_r", [1, 20])
    w_gate = din("w_gate", [NE, D, 512])
    w_up = din("w_up", [NE, D, 512])
    w_down = din("w_down", [NE, 512, D])
    c_ident = din("c_ident", [128, 128])
    c_rt = din("c_rt", [128, 128])
    c_tri = din("c_tri", [128, 128])
    c_swam = din("c_swam", [128, 256])
    c_cos = din("c_cos", [128, S], BF16)
    c_sin = din("c_sin", [128, S], BF16)
    out = nc.dram_tensor("out", [NSEQ, S, D], F32, kind="ExternalOutput").ap()
    mod_scr = nc.dram_tensor("mod_scr", [NSEQ, 6 * D], F32).ap()
    wsc_g = nc.dram_tensor("wsc_g", [NE, 128, 4096], BF16).ap()
    wsc_u = nc.dram_tensor("wsc_u", [NE, 128, 4096], BF16).ap()
    wsc_d = nc.dram_tensor("wsc_d", [NE, 128, 4096], BF16).ap()

    with ExitStack() as st:
        K = Sched(nc, st)
        PE, ACT, DVE, POOL, SP = K.pe, K.act, K.dve, K.pool, K.sp

        P0 = SB_BASE
        OW0 = P0 + 29 * KB
        X0 = OW0 + 48 * KB
        rP = Region(nc, P0, OW0, "P")
        ident_f = rP.alloc("ident_f", [128, 128], F32)
        ident_b = rP.alloc("ident_b", [128, 128], BF16)
        rt_b = rP.alloc("rt_b", [128, 128], BF16)
        tri_b = rP.alloc("tri_b", [128, 128], BF16)
        swam_b = rP.alloc("swam_b", [128, 256], BF16)
        ones_b = rP.alloc("ones_b", [128, 128], BF16)
        ones_f = rP.alloc("ones_f", [128, 128], F32)
        gfin_bc = rP.alloc("gfin_bc", [128, D], F32)
        wr_f = rP.alloc("wr_f", [128, 8, 20], F32)
        br_f = rP.alloc("br_f", [128, 20], F32)
        gds = rP.alloc("gds", [128, 1], F32)
        neglam = rP.alloc("neglam", [128, 1], F32)
        esink = rP.alloc("esink", [128, 4], F32)
        epsc = rP.alloc("epsc", [128, 1], F32)
        cact = rP.alloc("cact", [128, 8, NSEQ], F32)
        lamw = rP.alloc("lamw", [128, 256], F32)
        lamt = rP.alloc("lamt", [128, 8], F32)
        stat = rP.alloc("stat", [128, 64], F32)
        A1 = rP.alloc("A1", [128, D], F32)
        B1 = rP.alloc("B1", [128, D], F32)
        A2 = rP.alloc("A2", [128, D], F32)
        B2 = rP.alloc("B2", [128, D], F32)
        G2 = rP.alloc("G2", [128, D], F32)

        rOW = Region(nc, OW0, X0, "OW")
        oT = rOW.alloc("oT", [128, 8, S], BF16)
        wob = rOW.alloc("wob", [128, 8, D], BF16)

        rS1 = Region(nc, X0, SB_LIMIT, "S1")
        hT = rS1.alloc("hT", [128, 8, S], BF16)
        xs = [rS1.alloc(f"xs{i}", [128, D], F32) for i in range(3)]
        tmpf = rS1.alloc("tmpf", [128, D], F32)
        tmpfb = rS1.alloc("tmpfb", [128, D], F32)
        hb = rS1.alloc("hb", [128, D], BF16)
        hbb = rS1.alloc("hbb", [128, D], BF16)
        junk = rS1.alloc("junk", [128, D], BF16)
        rPro = Region(nc, X0, SB_LIMIT, "Pro")
        adst = [rPro.alloc(f"adst{i}", [128, 8, 512], F32) for i in range(2)]
        bst = [rPro.alloc(f"bst{i}", [128, 512], F32) for i in range(2)]
        modsb = [rPro.alloc(f"modsb{i}", [128, 512], F32) for i in range(2)]
        cstg = rPro.alloc("cstg", [128, 256], F32)


        rS2 = Region(nc, X0, SB_LIMIT, "S2")
        rS2.off += 8 * S * 2
        hT2 = hT
        dv = rS2.alloc("dv", [128, NT, 512], BF16)
        svd = rS2.alloc("svd", [128, NT, 2, 128], BF16)
        wstg = [rS2.alloc(f"wstg{i}", [128, 8, 256], F32) for i in range(2)]
        wub = [rS2.alloc(f"wub{i}", [128, 8, 256], BF16) for i in range(2)]
        qT = rS2.alloc("qT", [128, S], BF16)
        kz = [rS2.alloc(f"kz{i}", [128, S], BF16) for i in range(2)]
        cosb = rS2.alloc("cosb", [128, S], BF16)
        sinb = rS2.alloc("sinb", [128, S], BF16)
        pT = [rS2.alloc(f"pT{i}", [128, 512], BF16) for i in range(4)]
        qraw_s = [rS2.alloc(f"qraw{i}", [128, 512], BF16) for i in range(3)]
        rt1_s = [rS2.alloc(f"rt1_{i}", [128, 512], F32) for i in range(3)]
        rt2_s = [rS2.alloc(f"rt2_{i}", [128, 512], F32) for i in range(3)]
        tA = rS2.alloc("tA", [128, 512], F32)
        tB = rS2.alloc("tB", [128, 512], F32)
        tC = rS2.alloc("tC", [128, 512], F32)
        tD = rS2.alloc("tD", [128, 512], F32)
        sqb = rS2.alloc("sqb", [128, 512], BF16)
        svsb = rS2.alloc("svsb", [128, 128], BF16)

        rM = Region(nc, X0, SB_LIMIT, "M")
        acc = rM.alloc("acc", [128, NT, D], F32)
        xs5 = [rM.alloc(f"xs5_{i}", [128, D], F32) for i in range(3)]
        wostg = [rM.alloc(f"wostg{i}", [128, 2, D], F32) for i in range(2)]
        g1t = rM.alloc("g1t", [128, D], F32)
        rMo = Region(nc, OW0, X0, "Mo")
        wgb = [rMo.alloc(f"wgb{i}", [128, 8, 512], BF16) for i in range(2)]
        wubx = [rMo.alloc(f"wubx{i}", [128, 8, 512], BF16) for i in range(2)]
        wdb = [rMo.alloc(f"wdb{i}", [128, 4, D], BF16) for i in range(2)]
        rM2 = Region(nc, X0 + NT * D * 4, SB_LIMIT, "M2")
        h2T = rM2.alloc("h2T", [128, 8, TP], BF16)
        h2f = rM2.alloc("h2f", [128, D], F32)
        tmpf2 = rM2.alloc("tmpf2", [128, D], F32)
        h2Tf = [rM2.alloc(f"h2Tf{i}", [128, 8, 128], F32) for i in range(2)]
        junk2 = rM2.alloc("junk2", [128, D], BF16)
        ytmp = [rM2.alloc(f"ytmp{i}", [128, D], F32) for i in range(2)]
        estg = [rM2.alloc(f"estg{i}", [128, 1024], F32) for i in range(2)]
        aT = [rM2.alloc(f"aT{i}", [128, 512], BF16) for i in range(8)]
        sg = [rM2.alloc(f"sg{i}", [128, 512], BF16) for i in range(2)]
        lg = rM2.alloc("lg", [128, NTP, 20], F32)
        comb = rM2.alloc("comb", [128, NTP, 16], F32)
        rw = rM2.alloc("rw", [128, 16, NTP, 4], F32)
        rs = rM2.alloc("rs", [128, 16, NTP], F32)

        ps = [st.enter_context(nc.psum_tensor(f"ps{i}", [128, 1024], F32)) for i in range(4)]
        PB = [K.buf(f"pb{i}") for i in range(8)]

        def bank(i):
            return ps[i // 2][:, (i % 2) * 512:(i % 2 + 1) * 512]

        def mk(*names):
            return {n: K.buf(n) for n in names}

        b = mk("ident_f", "ident_b", "rt_b", "tri_b", "swam_b", "ones_b", "ones_f", "gfin_bc", "wr_f", "br_f",
               "gds", "neglam", "esink", "cact", "lamw", "lamt", "stat", "A1", "B1", "A2", "B2", "G2",
               "wob", "hT", "tmpf", "hb", "junk", "cstg", "dv", "svd", "qT", "kz0", "kz1", "cosb", "sinb",
               "qraw", "rt1", "rt2", "tA", "tB", "tC", "tD", "epsc", "sqb", "svsb", "g1t", "h2T", "h2f", "tmpf2", "junk2",
               "lg", "comb", "rw", "rs", "mod_scr")
        b_xs = [K.buf(f"xs{i}") for i in range(3)]
        b_tmpf2s = [b["tmpf"], K.buf("tmpfb")]
        b_hb2s = [b["hb"], K.buf("hbb")]
        b_st = [K.buf(f"st{i}") for i in range(8)]
        b_adst = [K.buf(f"adst{i}") for i in range(2)]
        b_bst = [K.buf(f"bst{i}") for i in range(2)]
        b_modsb = [K.buf(f"modsb{i}") for i in range(2)]
        b_wstg = [K.buf(f"wstg{i}") for i in range(2)]
        b_qraw = [K.buf(f"qraw{i}") for i in range(3)]
        b_rt1 = [K.buf(f"rt1_{i}") for i in range(3)]
        b_rt2 = [K.buf(f"rt2_{i}") for i in range(3)]
        b_wub = [K.buf(f"wub{i}") for i in range(2)]
        b_pT = [K.buf(f"pT{i}") for i in range(4)]
        b_oT = [K.buf(f"oT{i}") for i in range(8)]
        b_acc = [K.buf(f"acc{i}") for i in range(NT)]
        b_xs5 = [K.buf(f"xs5_{i}") for i in range(3)]
        b_wostg = [K.buf(f"wostg{i}") for i in range(2)]
        b_wgb = [K.buf(f"wgb{i}") for i in range(2)]
        b_wubx = [K.buf(f"wubx{i}") for i in range(2)]
        b_wdb = [K.buf(f"wdb{i}") for i in range(2)]
        b_h2Tf = [K.buf(f"h2Tf{i}") for i in range(2)]
        b_ytmp = [K.buf(f"ytmp{i}") for i in range(2)]
        b_estg = [K.buf(f"estg{i}") for i in range(2)]
        b_wsc = [K.buf(f"wsc{i}") for i in range(NE)]
        b_aT = [K.buf(f"aT{i}") for i in range(8)]
        b_sg = [K.buf(f"sg{i}") for i in range(2)]

        s_const = K.new_dma_sem("const")
        s_x = [K.new_dma_sem(f"x{i}") for i in range(3)]
        s_w = [K.new_dma_sem(f"w{i}") for i in range(2)]
        s_mod = K.new_dma_sem("mod")
        s_misc = K.new_dma_sem("misc")
        s_out = K.new_dma_sem("out")

        def _ck(name):
            if STOP == name:
                raise _Stop()

        try:
            def load_const_cast(dst, dst_buf, src, width):
                K.dma(SP, cstg[:, 0:width], src, None, writes=[b["cstg"]])
                K.op(DVE, lambda e: e.tensor_copy(out=dst[:], in_=cstg[:, 0:width]), reads=[b["cstg"]], writes=[dst_buf])

            K.dma(SP, ident_f[:], c_ident, None, writes=[b["ident_f"]])
            K.op(DVE, lambda e: e.tensor_copy(out=ident_b[:], in_=ident_f[:]), reads=[b["ident_f"]], writes=[b["ident_b"]])
            load_const_cast(rt_b, b["rt_b"], c_rt, 128)
            load_const_cast(tri_b, b["tri_b"], c_tri, 128)
            load_const_cast(swam_b, b["swam_b"], c_swam, 256)
            K.op(POOL, lambda e: e.memset(ones_b[:], 1.0), writes=[b["ones_b"]])
            K.op(POOL, lambda e: e.memset(ones_f[:], 1.0), writes=[b["ones_f"]])
            K.op(POOL, lambda e: e.memset(epsc[:], EPS), writes=[b["epsc"]])
            K.dma(SP, gfin_bc[:], g_fin.partition_broadcast(128), None, writes=[b["gfin_bc"]])
            K.dma(SP, wr_f[:], w_r.rearrange("(k p) n -> p k n", p=128), None, writes=[b["wr_f"]])
            K.dma(SP, br_f[:], b_r.partition_broadcast(128), None, writes=[b["br_f"]])
            K.dma(SP, gds[:], gds_c, None, writes=[b["gds"]])
            K.dma(SP, esink[:], sink_c, None, writes=[b["esink"]])
            K.dma(SP, cact[:], cT, None, writes=[b["cact"]])
            K.dma(SP, lamw[:], dlam.partition_broadcast(128), None, writes=[b["lamw"]])

            K.op(DVE, lambda e: e.tensor_scalar(out=gds[:], in0=gds[:], scalar1=0.8, scalar2=None, op0=ALU.mult),
                 reads=[b["gds"]], writes=[b["gds"]])
            K.op(ACT, lambda e: e.activation(out=esink[:], in_=esink[:], func=AF.Exp), reads=[b["esink"]], writes=[b["esink"]])
            K.op(DVE, lambda e: e.tensor_tensor(out=lamw[:, 0:64], in0=lamw[:, 0:64], in1=lamw[:, 64:128], op=ALU.mult),
                 reads=[b["lamw"]], writes=[b["lamw"]])
            K.op(DVE, lambda e: e.tensor_tensor(out=lamw[:, 128:192], in0=lamw[:, 128:192], in1=lamw[:, 192:256], op=ALU.mult),
                 reads=[b["lamw"]], writes=[b["lamw"]])
            K.op(DVE, lambda e: e.reduce_sum(out=lamt[:, 0:1], in_=lamw[:, 0:64], axis=AX.X), reads=[b["lamw"]], writes=[b["lamt"]])
            K.op(DVE, lambda e: e.reduce_sum(out=lamt[:, 1:2], in_=lamw[:, 128:192], axis=AX.X), reads=[b["lamw"]], writes=[b["lamt"]])
            K.op(ACT, lambda e: e.activation(out=lamt[:, 2:4], in_=lamt[:, 0:2], func=AF.Exp), reads=[b["lamt"]], writes=[b["lamt"]])
            K.op(DVE, lambda e: e.scalar_tensor_tensor(out=neglam[:], in0=lamt[:, 3:4], scalar=-0.2, in1=lamt[:, 2:3],
                                                       op0=ALU.add, op1=ALU.subtract),
                 reads=[b["lamt"]], writes=[b["neglam"]])
            K.op(ACT, lambda e: e.activation(out=cact[:], in_=cact[:], func=AF.Silu), reads=[b["cact"]], writes=[b["cact"]])

            for n in range(12):
                sl = n % 2
                K.dma(SP, adst[sl][:], w_ada[:, n * 512:(n + 1) * 512].rearrange("(k p) n -> p k n", p=128), None,
                      writes=[b_adst[sl]])
                K.dma(SP, bst[sl][0:NSEQ, :], b_ada[:, n * 512:(n + 1) * 512].partition_broadcast(NSEQ), None, writes=[b_bst[sl]])
                pb = n % 2
                for k in range(8):
                    K.op(PE, lambda e, k=k, sl=sl, pb=pb: e.matmul(bank(pb)[0:NSEQ, :], lhsT=cact[:, k, :], rhs=adst[sl][:, k, :],
                                                                   start=(k == 0), stop=(k == 7)),
                         reads=[b["cact"], b_adst[sl]], writes=[PB[pb]], inc=(k == 7))
                K.op(DVE, lambda e, sl=sl, pb=pb: e.tensor_tensor(out=modsb[sl][0:NSEQ, :], in0=bank(pb)[0:NSEQ, :], in1=bst[sl][0:NSEQ, :], op=ALU.add),
                     reads=[PB[pb], b_bst[sl]], writes=[b_modsb[sl]])
                K.dma(SP, mod_scr[:, n * 512:(n + 1) * 512], modsb[sl][0:NSEQ, :], s_mod, reads=[b_modsb[sl]], writes=[b["mod_scr"]])
            K.barrier()
            _ck('pro')

            def rstd_from_ss(ss_ap, scale, sbuf=None):
                sbuf = sbuf if sbuf is not None else b["stat"]
                K.op(ACT, lambda e: e.activation(out=ss_ap, in_=ss_ap, func=AF.Sqrt, scale=scale, bias=EPS),
                     reads=[sbuf], writes=[sbuf])
                K.op(DVE, lambda e: e.reciprocal(out=ss_ap, in_=ss_ap), reads=[sbuf], writes=[sbuf])

            def load_mod_vec(dst, dst_buf, seq, v, sem=None):
                K.dma(SP, dst[:], mod_scr[seq:seq + 1, v * D:(v + 1) * D].partition_broadcast(128), None,
                      reads=[b["mod_scr"]], writes=[dst_buf])

            for seq in range(NSEQ):
                load_mod_vec(B1, b["B1"], seq, 0)
                load_mod_vec(A1, b["A1"], seq, 1)
                K.dma(SP, tmpf[:], g_mix.partition_broadcast(128), None, writes=[b["tmpf"]])
                K.op(DVE, lambda e: e.scalar_tensor_tensor(out=A1[:], in0=A1[:], scalar=1.0, in1=tmpf[:], op0=ALU.add, op1=ALU.mult),
                     reads=[b["A1"], b["tmpf"]], writes=[b["A1"]])

                tmpf_s = [tmpf, tmpfb]
                hb_s = [hb, hbb]

                def s1_stage1(t):
                    sl = t % 3
                    d2 = t % 2
                    K.dma(SP, xs[sl][:], x[seq, t * 128:(t + 1) * 128, :], None, writes=[b_xs[sl]])
                    ssap = stat[:, 8 + d2:9 + d2]
                    K.op(ACT, lambda e: e.activation(out=junk[:], in_=xs[sl][:], func=AF.Square, accum_out=ssap),
                         reads=[b_xs[sl]], writes=[b_st[d2]])
                    rstd_from_ss(ssap, 1.0 / D, b_st[d2])
                    K.op(DVE, lambda e: e.scalar_tensor_tensor(out=tmpf_s[d2][:], in0=xs[sl][:], scalar=ssap, in1=A1[:],
                                                               op0=ALU.mult, op1=ALU.mult),
                         reads=[b_xs[sl], b_st[d2], b["A1"]], writes=[b_tmpf2s[d2]])
                    K.op(POOL, lambda e: e.tensor_tensor(out=hb_s[d2][:], in0=tmpf_s[d2][:], in1=B1[:], op=ALU.add),
                         reads=[b_tmpf2s[d2], b["B1"]], writes=[b_hb2s[d2]])

                def s1_stage2(t):
                    d2 = t % 2
                    pbi = t % 2
                    pbv = bank(pbi).bitcast(BF16)
                    for k in range(8):
                        K.op(PE, lambda e, k=k: e.transpose(out=pbv[:, k * 128:(k + 1) * 128], in_=hb_s[d2][:, k * 128:(k + 1) * 128],
                                                            identity=ident_b[:]),
                             reads=[b_hb2s[d2], b["ident_b"]], writes=[PB[pbi]], inc=(k == 7))
                    K.op(ACT, lambda e: e.copy(out=hT[:, :, t * 128:(t + 1) * 128], in_=pbv.rearrange("p (k c) -> p k c", k=8)),
                         reads=[PB[pbi]], writes=[b["hT"]])

                for t in range(NT + 1):
                    if t < NT:
                        s1_stage1(t)
                    if t >= 1:
                        s1_stage2(t - 1)
                K.barrier()
                _ck('b1')

                K.dma(SP, cosb[:], c_cos, None, writes=[b["cosb"]])
                K.dma(SP, sinb[:], c_sin, None, writes=[b["sinb"]])
                K.op(POOL, lambda e: e.memset(kz[0][:], 0.0), writes=[b["kz0"]])
                K.op(POOL, lambda e: e.memset(kz[1][:], 0.0), writes=[b["kz1"]])

                wcnt = [0]

                def load_wunit(col0, ncols):
                    sl = wcnt[0] % 2
                    wcnt[0] += 1
                    K.dma(SP, wstg[sl][:, :, 0:ncols], w_in[:, col0:col0 + ncols].rearrange("(k p) n -> p k n", p=128), None,
                          writes=[b_wstg[sl]])
                    K.op(POOL, lambda e, sl=sl: e.tensor_copy(out=wub[sl][:, :, 0:ncols], in_=wstg[sl][:, :, 0:ncols]),
                         reads=[b_wstg[sl]], writes=[b_wub[sl]])
                    return sl

                for piece, (c0, ncol) in enumerate([(2048, 256), (2304, 256), (2560, 128)]):
                    sl = load_wunit(c0, ncol)
                    for t in range(NT):
                        pbi = t % 2
                        for k in range(8):
                            K.op(PE, lambda e, k=k, t=t, sl=sl, pbi=pbi, ncol=ncol: e.matmul(
                                bank(pbi)[:, 0:ncol], lhsT=hT2[:, k, t * 128:(t + 1) * 128], rhs=wub[sl][:, k, 0:ncol],
                                start=(k == 0), stop=(k == 7)),
                                reads=[b["hT"], b_wub[sl]], writes=[PB[pbi]], inc=(k == 7))
                        if piece < 2:
                            K.op(ACT, lambda e, t=t, pbi=pbi, piece=piece: e.copy(out=dv[:, t, piece * 256:(piece + 1) * 256],
                                                                                  in_=bank(pbi)[:, 0:256]),
                                 reads=[PB[pbi]], writes=[b["dv"]])
                        else:
                            K.op(ACT, lambda e, pbi=pbi: e.copy(out=svsb[:], in_=bank(pbi)[:, 0:128]), reads=[PB[pbi]], writes=[b["svsb"]])
                            for hk in range(2):
                                for dup in range(2):
                                    K.op(POOL, lambda e, t=t, hk=hk, dup=dup: e.tensor_copy(
                                        out=svd[:, t, hk, dup * 64:(dup + 1) * 64], in_=svsb[:, hk * 64:(hk + 1) * 64]),
                                        reads=[b["svsb"]], writes=[b["svd"]])

                _ck('v')
                sl_next = load_wunit(0, 256)
                for u in range(8):
                    sl = sl_next
                    def rope_a(ridx):
                        j, which = divmod(ridx, 2)
                        cs = slice(j * 512, (j + 1) * 512)
                        pbi = (0, 1, 2, 7)[ridx % 4]
                        rsl = ridx % 3
                        for k in range(8):
                            K.op(PE, lambda e, k=k: e.matmul(
                                bank(pbi), lhsT=wub[sl][:, k, which * 128:(which + 1) * 128], rhs=hT2[:, k, cs],
                                start=(k == 0), stop=(k == 7)),
                                reads=[b["hT"], b_wub[sl]], writes=[PB[pbi]], inc=(k == 7))
                        qraw = qraw_s[rsl]
                        K.op(ACT, lambda e: e.copy(out=qraw[:], in_=bank(pbi)), reads=[PB[pbi]], writes=[b_qraw[rsl], PB[pbi]])

                    def rope_b(ridx):
                        j, which = divmod(ridx, 2)
                        cs = slice(j * 512, (j + 1) * 512)
                        pbi = (0, 1, 2, 7)[ridx % 4]
                        rb = (3, 4, 5, 6)[ridx % 4]
                        rsl = ridx % 3
                        qraw, rt1, rt2 = qraw_s[rsl], rt1_s[rsl], rt2_s[rsl]
                        bq, b1_, b2_ = b_qraw[rsl], b_rt1[rsl], b_rt2[rsl]
                        K.op(PE, lambda e: e.matmul(bank(rb), lhsT=rt_b[:], rhs=qraw[:], start=True, stop=True),
                             reads=[b["rt_b"], bq], writes=[PB[rb]])
                        K.op(DVE, lambda e: e.tensor_tensor(out=rt1[:], in0=bank(pbi), in1=cosb[:, cs], op=ALU.mult),
                             reads=[PB[pbi], b["cosb"]], writes=[b1_, PB[pbi]])
                        K.op(DVE, lambda e: e.tensor_tensor(out=rt2[:], in0=bank(rb), in1=sinb[:, cs], op=ALU.mult),
                             reads=[PB[rb], b["sinb"]], writes=[b2_])
                        if which == 0:
                            K.op(POOL, lambda e: e.tensor_tensor(out=qT[:, cs], in0=rt1[:], in1=rt2[:], op=ALU.add),
                                 reads=[b1_, b2_], writes=[b["qT"]])
                        else:
                            K.op(DVE, lambda e: e.tensor_tensor(out=kz[0][0:64, cs], in0=rt1[0:64, :], in1=rt2[0:64, :], op=ALU.add),
                                 reads=[b1_, b2_], writes=[b["kz0"]])
                            K.op(DVE, lambda e: e.tensor_tensor(out=kz[1][64:128, cs], in0=rt1[64:128, :], in1=rt2[64:128, :], op=ALU.add),
                                 reads=[b1_, b2_], writes=[b["kz1"]])

                    nrope = 2 * NB
                    for ridx in range(nrope + 1):
                        if ridx < nrope:
                            rope_a(ridx)
                        if ridx >= 1:
                            rope_b(ridx - 1)

                    _ck(f'proj{u}')
                    if u < 7:
                        sl_next = load_wunit((u + 1) * 256, 256)
                    if u < 4:
                        h = u
                        for j in range(NB):
                            items = [(c, m) for c in range(4 * j + 4) for m in range(2)]
                            nit = len(items)
                            last_c = 4 * j + 3

                            def s_stage(idx):
                                c, m = items[idx]
                                i = c - 4 * j
                                q0 = max(i, 0) * 128
                                sb_i = (0, 1, 2, 7)[idx % 4]
                                pt_i = idx % 4
                                K.op(PE, lambda e, c=c, m=m, q0=q0, sb_i=sb_i: e.matmul(
                                    bank(sb_i)[:, q0:512], lhsT=kz[m][:, c * 128:(c + 1) * 128], rhs=qT[:, j * 512 + q0:(j + 1) * 512],
                                    start=True, stop=True),
                                    reads=[b["kz0"], b["kz1"], b["qT"]], writes=[PB[sb_i]])
                                K.op(ACT, lambda e, q0=q0, sb_i=sb_i, pt_i=pt_i: e.activation(
                                    out=pT[pt_i][:, q0:512], in_=bank(sb_i)[:, q0:512], func=AF.Exp, scale=0.125),
                                    reads=[PB[sb_i]], writes=[b_pT[pt_i]])
                                if i >= 0:
                                    K.op(DVE, lambda e, q0=q0, pt_i=pt_i: e.tensor_tensor(
                                        out=pT[pt_i][:, q0:q0 + 128], in0=pT[pt_i][:, q0:q0 + 128], in1=tri_b[:], op=ALU.mult),
                                        reads=[b_pT[pt_i], b["tri_b"]], writes=[b_pT[pt_i]])

                            def pv_stage(idx):
                                c, m = items[idx]
                                i = c - 4 * j
                                q0 = max(i, 0) * 128
                                pt_i = idx % 4
                                ob = 3 + m
                                lb = 5 + m
                                K.op(PE, lambda e, c=c, q0=q0, pt_i=pt_i, ob=ob: e.matmul(
                                    bank(ob)[:, q0:512], lhsT=dv[:, c, h * 128:(h + 1) * 128], rhs=pT[pt_i][:, q0:512],
                                    start=(c == 0), stop=(c == last_c)),
                                    reads=[b["dv"], b_pT[pt_i]], writes=[PB[ob]], inc=False)
                                K.op(PE, lambda e, c=c, q0=q0, pt_i=pt_i, lb=lb: e.matmul(
                                    bank(lb)[:, q0:512], lhsT=ones_b[:], rhs=pT[pt_i][:, q0:512],
                                    start=(c == 0), stop=(c == last_c)),
                                    reads=[b["ones_b"], b_pT[pt_i]], writes=[PB[lb]])

                            for idx in range(nit + 3):
                                if idx < nit:
                                    s_stage(idx)
                                if idx >= 3:
                                    pv_stage(idx - 3)
                            cs = slice(j * 512, (j + 1) * 512)
                            K.op(ACT, lambda e: e.activation(out=tA[:], in_=bank(5), func=AF.Ln), reads=[PB[5]], writes=[b["tA"]])
                            K.op(DVE, lambda e: e.tensor_copy(out=tB[:], in_=bank(3)), reads=[PB[3]], writes=[b["tB"]])
                            K.op(ACT, lambda e: e.activation(out=tD[:], in_=bank(6), func=AF.Ln), reads=[PB[6]], writes=[b["tD"]])
                            K.op(DVE, lambda e: e.tensor_copy(out=tC[:], in_=bank(4)), reads=[PB[4]], writes=[b["tC"]])
                            K.op(ACT, lambda e: e.activation(out=tA[:], in_=tA[:], func=AF.Exp, scale=-1.0), reads=[b["tA"]], writes=[b["tA"]])
                            K.op(ACT, lambda e: e.activation(out=tD[:], in_=tD[:], func=AF.Exp, scale=-1.0), reads=[b["tD"]], writes=[b["tD"]])
                            K.op(DVE, lambda e: e.tensor_tensor(out=tB[:], in0=tB[:], in1=tA[:], op=ALU.mult),
                                 reads=[b["tB"], b["tA"]], writes=[b["tB"]])
                            K.op(DVE, lambda e: e.tensor_tensor(out=tC[:], in0=tC[:], in1=tD[:], op=ALU.mult),
                                 reads=[b["tC"], b["tD"]], writes=[b["tC"]])
                            K.op(DVE, lambda e: e.scalar_tensor_tensor(out=tB[:], in0=tC[:], scalar=neglam[:, 0:1], in1=tB[:],
                                                                       op0=ALU.mult, op1=ALU.add),
                                 reads=[b["tC"], b["tB"], b["neglam"]], writes=[b["tB"]])
                            K.op(ACT, lambda e: e.activation(out=sqb[:], in_=tB[:], func=AF.Square), reads=[b["tB"]], writes=[b["sqb"]])
                            K.op(PE, lambda e: e.matmul(bank(7), lhsT=ones_b[:], rhs=sqb[:], start=True, stop=True),
                                 reads=[b["ones_b"], b["sqb"]], writes=[PB[7]])
                            K.op(ACT, lambda e: e.activation(out=tA[:], in_=bank(7), func=AF.Ln, scale=1.0 / 128, bias=epsc[:, 0:1]),
                                 reads=[PB[7], b["epsc"]], writes=[b["tA"]])
                            K.op(ACT, lambda e: e.activation(out=tA[:], in_=tA[:], func=AF.Exp, scale=-0.5), reads=[b["tA"]], writes=[b["tA"]])
                            K.op(DVE, lambda e, cs=cs: e.scalar_tensor_tensor(out=oT[:, h, cs], in0=tB[:], scalar=gds[:, 0:1], in1=tA[:],
                                                                              op0=ALU.mult, op1=ALU.mult),
                                 reads=[b["tB"], b["gds"], b["tA"]], writes=[b_oT[h]])
                    else:
                        ch = u - 4
                        hk = ch // 2
                        for j in range(NB):
                            items = list(range(4 * j, 4 * j + 4))
                            nit = len(items)

                            def s_stage(idx):
                                t = items[idx]
                                sb_i = (0, 1, 2, 7)[idx % 4]
                                pt_i = idx % 4
                                lo = 0 if t > 0 else 128
                                for s in range(2):
                                    c0 = s * 256
                                    if t > 0:
                                        K.op(PE, lambda e, c0=c0, s=s: e.matmul(
                                            bank(sb_i)[:, c0:c0 + 128], lhsT=kz[s][:, (t - 1) * 128:t * 128], rhs=qT[:, t * 128:(t + 1) * 128],
                                            start=True, stop=True),
                                            reads=[b["kz0"], b["kz1"], b["qT"]], writes=[PB[sb_i]], inc=False)
                                    K.op(PE, lambda e, c0=c0, s=s: e.matmul(
                                        bank(sb_i)[:, c0 + 128:c0 + 256], lhsT=kz[s][:, t * 128:(t + 1) * 128], rhs=qT[:, t * 128:(t + 1) * 128],
                                        start=True, stop=True),
                                        reads=[b["kz0"], b["kz1"], b["qT"]], writes=[PB[sb_i]], inc=(s == 1))
                                src3 = bank(sb_i).rearrange("p (s c) -> p s c", s=2)[:, :, lo:256]
                                dst3 = pT[pt_i][:].rearrange("p (s c) -> p s c", s=2)[:, :, lo:256]
                                msk3 = swam_b[:, lo:256].unsqueeze(1).broadcast_to([128, 2, 256 - lo])
                                K.op(ACT, lambda e: e.activation(out=dst3, in_=src3, func=AF.Exp, scale=0.125),
                                     reads=[PB[sb_i]], writes=[b_pT[pt_i]])
                                K.op(DVE, lambda e: e.tensor_tensor(out=dst3, in0=dst3, in1=msk3, op=ALU.mult),
                                     reads=[b_pT[pt_i], b["swam_b"]], writes=[b_pT[pt_i]])

                            def pv_stage(idx):
                                t = items[idx]
                                pt_i = idx % 4
                                tq = t - 4 * j
                                reg = slice(tq * 128, (tq + 1) * 128)
                                for s in range(2):
                                    c0 = s * 256
                                    ob = 3 + s
                                    lb = 5 + s
                                    first = True
                                    if t > 0:
                                        K.op(PE, lambda e, c0=c0, ob=ob: e.matmul(
                                            bank(ob)[:, reg], lhsT=svd[:, t - 1, hk, :], rhs=pT[pt_i][:, c0:c0 + 128], start=True, stop=False),
                                            reads=[b["svd"], b_pT[pt_i]], writes=[PB[ob]], inc=False)
                                        first = False
                                    K.op(PE, lambda e, c0=c0, ob=ob, first=first: e.matmul(
                                        bank(ob)[:, reg], lhsT=svd[:, t, hk, :], rhs=pT[pt_i][:, c0 + 128:c0 + 256], start=first, stop=True),
                                        reads=[b["svd"], b_pT[pt_i]], writes=[PB[ob]], inc=False)
                                    if t > 0:
                                        K.op(PE, lambda e, c0=c0, lb=lb: e.matmul(
                                            bank(lb)[:, reg], lhsT=ones_b[:], rhs=pT[pt_i][:, c0:c0 + 128], start=True, stop=False),
                                            reads=[b["ones_b"], b_pT[pt_i]], writes=[PB[lb]], inc=False)
                                    K.op(PE, lambda e, c0=c0, lb=lb, first=first: e.matmul(
                                        bank(lb)[:, reg], lhsT=ones_b[:], rhs=pT[pt_i][:, c0 + 128:c0 + 256], start=first, stop=True),
                                        reads=[b["ones_b"], b_pT[pt_i]], writes=[PB[lb]], inc=(s == 1))

                            for idx in range(nit + 3):
                                if idx < nit:
                                    s_stage(idx)
                                if idx >= 3:
                                    pv_stage(idx - 3)
                            cs = slice(j * 512, (j + 1) * 512)
                            for s in range(2):
                                pr = slice(s * 64, (s + 1) * 64)
                                K.op(DVE, lambda e, s=s, pr=pr: e.tensor_scalar(out=tA[pr, :], in0=bank(5 + s)[pr, :], scalar1=esink[pr, ch:ch + 1],
                                                                                scalar2=None, op0=ALU.add),
                                     reads=[PB[5 + s], b["esink"]], writes=[b["tA"]])
                                K.op(ACT, lambda e, pr=pr: e.activation(out=tA[pr, :], in_=tA[pr, :], func=AF.Ln), reads=[b["tA"]], writes=[b["tA"]])
                                K.op(ACT, lambda e, pr=pr: e.activation(out=tA[pr, :], in_=tA[pr, :], func=AF.Exp, scale=-1.0), reads=[b["tA"]], writes=[b["tA"]])
                                K.op(DVE, lambda e, s=s, pr=pr, cs=cs: e.tensor_tensor(out=oT[pr, 4 + ch, cs], in0=bank(3 + s)[pr, :], in1=tA[pr, :],
                                                                                       op=ALU.mult),
                                     reads=[PB[3 + s], b["tA"]], writes=[b_oT[4 + ch]])
                    _ck(f'unit{u}')
                K.barrier()
                _ck('b2')

                load_mod_vec(g1t, b["g1t"], seq, 2)
                for i in range(4):
                    sl = i % 2
                    K.dma(SP, wostg[sl][:], w_out[i * 256:(i + 1) * 256, :].rearrange("(k p) n -> p k n", p=128), None,
                          writes=[b_wostg[sl]])
                    for kk in range(2):
                        K.op(DVE, lambda e, i=i, kk=kk, sl=sl: e.tensor_tensor(out=wob[:, 2 * i + kk, :], in0=wostg[sl][:, kk, :], in1=g1t[:],
                                                                               op=ALU.mult),
                             reads=[b_wostg[sl], b["g1t"]], writes=[b["wob"]])
                for t in range(NT):
                    sl = t % 3
                    K.dma(SP, xs5[sl][:], x[seq, t * 128:(t + 1) * 128, :], None, writes=[b_xs5[sl]])
                    pp = t % 2
                    for half in range(2):
                        for kk in range(8):
                            K.op(PE, lambda e, t=t, kk=kk, half=half, pp=pp: e.matmul(
                                bank(2 * pp + half), lhsT=oT[:, kk, t * 128:(t + 1) * 128], rhs=wob[:, kk, half * 512:(half + 1) * 512],
                                start=(kk == 0), stop=(kk == 7)),
                                reads=[b_oT[kk], b["wob"]], writes=[PB[2 * pp + half]], inc=(kk == 7))
                    K.op(DVE, lambda e, t=t, sl=sl, pp=pp: e.tensor_tensor(out=acc[:, t, :], in0=ps[pp][:], in1=xs5[sl][:], op=ALU.add),
                         reads=[PB[2 * pp], PB[2 * pp + 1], b_xs5[sl]], writes=[b_acc[t]])
                K.barrier()
                _ck('b3')

                load_mod_vec(B2, b["B2"], seq, 3)
                load_mod_vec(A2, b["A2"], seq, 4)
                load_mod_vec(G2, b["G2"], seq, 5)
                K.dma(SP, tmpf2[:], g_ffn.partition_broadcast(128), None, writes=[b["tmpf2"]])
                K.op(DVE, lambda e: e.scalar_tensor_tensor(out=A2[:], in0=A2[:], scalar=1.0, in1=tmpf2[:], op0=ALU.add, op1=ALU.mult),
                     reads=[b["A2"], b["tmpf2"]], writes=[b["A2"]])

                for p in range(NPASS):
                    tmpX = [tmpf2, ytmp[0]]
                    b_tmpX = [b["tmpf2"], b_ytmp[0]]
                    h2X = [h2f, ytmp[1]]
                    b_h2X = [b["h2f"], b_ytmp[1]]

                    def s6_st1(tt):
                        t = p * NTP + tt
                        d2 = tt % 2
                        ssap = stat[:, 10 + d2:11 + d2]
                        K.op(ACT, lambda e: e.activation(out=junk2[:], in_=acc[:, t, :], func=AF.Square, accum_out=ssap),
                             reads=[b_acc[t]], writes=[b_st[2 + d2]])
                        rstd_from_ss(ssap, 1.0 / D, b_st[2 + d2])
                        K.op(DVE, lambda e: e.scalar_tensor_tensor(out=tmpX[d2][:], in0=acc[:, t, :], scalar=ssap, in1=A2[:],
                                                                   op0=ALU.mult, op1=ALU.mult),
                             reads=[b_acc[t], b_st[2 + d2], b["A2"]], writes=[b_tmpX[d2]])
                        K.op(POOL, lambda e: e.tensor_tensor(out=h2X[d2][:], in0=tmpX[d2][:], in1=B2[:], op=ALU.add),
                             reads=[b_tmpX[d2], b["B2"]], writes=[b_h2X[d2]])

                    def s6_st2(tt):
                        d2 = tt % 2
                        pp = tt % 2
                        fs = tt % 2
                        for k in range(8):
                            K.op(PE, lambda e, k=k: e.transpose(out=ps[pp][:, k * 128:(k + 1) * 128], in_=h2X[d2][:, k * 128:(k + 1) * 128],
                                                                identity=ident_f[:]),
                                 reads=[b_h2X[d2], b["ident_f"]], writes=[PB[2 * pp], PB[2 * pp + 1]], inc=(k == 7))
                        K.op(DVE, lambda e: e.tensor_copy(out=h2Tf[fs][:], in_=ps[pp][:].rearrange("p (k c) -> p k c", k=8)),
                             reads=[PB[2 * pp], PB[2 * pp + 1]], writes=[b_h2Tf[fs]])
                        K.op(ACT, lambda e: e.copy(out=h2T[:, :, tt * 128:(tt + 1) * 128], in_=h2Tf[fs][:]),
                             reads=[b_h2Tf[fs]], writes=[b["h2T"]])

                    def s6_st3(tt):
                        fs = tt % 2
                        lb = 4 + tt % 2
                        for k in range(8):
                            K.op(PE, lambda e, k=k: e.matmul(bank(lb)[:, 0:20], lhsT=h2Tf[fs][:, k, :], rhs=wr_f[:, k, :],
                                                             start=(k == 0), stop=(k == 7)),
                                 reads=[b_h2Tf[fs], b["wr_f"]], writes=[PB[lb]], inc=(k == 7))
                        K.op(DVE, lambda e: e.tensor_tensor(out=lg[:, tt, :], in0=bank(lb)[:, 0:20], in1=br_f[:], op=ALU.add),
                             reads=[PB[lb], b["br_f"]], writes=[b["lg"]])

                    for i in range(NTP + 2):
                        if i < NTP:
                            s6_st1(i)
                        if 1 <= i <= NTP:
                            s6_st2(i - 1)
                        if i >= 2:
                            s6_st3(i - 2)

                    RW, RS = [b["rw"]], [b["rs"]]

                    def bc(ap2):
                        return ap2.unsqueeze(2).broadcast_to([128, NTP, 4])

                    def vop(fn, reads, writes):
                        K.op(DVE, fn, reads=reads, writes=writes)

                    LGg = lg[:, :, 0:4]
                    gmax, gsum, gp, m1, m2, dd, ee, w1, w2 = [rs[:, i, :] for i in range(9)]
                    goh, gsh, ig, tmp4, oh1, ig2, oh2, within, gw = [rw[:, i, :, :] for i in range(9)]
                    vop(lambda e: e.tensor_reduce(out=gmax, in_=LGg, axis=AX.X, op=ALU.max), [b["lg"]], RS)
                    vop(lambda e: e.tensor_tensor(out=goh, in0=LGg, in1=bc(gmax), op=ALU.is_equal), [b["lg"], b["rs"]], RW)
                    vop(lambda e: e.tensor_tensor(out=gsh, in0=LGg, in1=bc(gmax), op=ALU.subtract), [b["lg"], b["rs"]], RW)
                    K.op(ACT, lambda e: e.activation(out=gsh, in_=gsh, func=AF.Exp), reads=RW, writes=RW)
                    vop(lambda e: e.tensor_reduce(out=gsum, in_=gsh, axis=AX.X, op=ALU.add), RW, RS)
                    vop(lambda e: e.reciprocal(out=gp, in_=gsum), RS, RS)
                    for g in range(4):
                        Lg = lg[:, :, 4 + 4 * g:8 + 4 * g]
                        gsel = rw[:, 0, :, g:g + 1].broadcast_to([128, NTP, 4])
                        if g == 0:
                            vop(lambda e, Lg=Lg, gsel=gsel: e.tensor_tensor(out=ig, in0=Lg, in1=gsel, op=ALU.mult), [b["lg"], b["rw"]], RW)
                        else:
                            vop(lambda e, Lg=Lg, gsel=gsel: e.tensor_tensor(out=tmp4, in0=Lg, in1=gsel, op=ALU.mult), [b["lg"], b["rw"]], RW)
                            vop(lambda e: e.tensor_tensor(out=ig, in0=ig, in1=tmp4, op=ALU.add), RW, RW)
                    vop(lambda e: e.tensor_reduce(out=m1, in_=ig, axis=AX.X, op=ALU.max), RW, RS)
                    vop(lambda e: e.tensor_tensor(out=oh1, in0=ig, in1=bc(m1), op=ALU.is_equal), RW + RS, RW)
                    vop(lambda e: e.scalar_tensor_tensor(out=ig2, in0=oh1, scalar=-1e30, in1=ig, op0=ALU.mult, op1=ALU.add), RW, RW)
                    vop(lambda e: e.tensor_reduce(out=m2, in_=ig2, axis=AX.X, op=ALU.max), RW, RS)
                    vop(lambda e: e.tensor_tensor(out=oh2, in0=ig2, in1=bc(m2), op=ALU.is_equal), RW + RS, RW)
                    vop(lambda e: e.tensor_tensor(out=dd, in0=m2, in1=m1, op=ALU.subtract), RS, RS)
                    K.op(ACT, lambda e: e.activation(out=ee, in_=dd, func=AF.Exp), reads=RS, writes=RS)
                    vop(lambda e: e.tensor_scalar(out=dd, in0=ee, scalar1=1.0, scalar2=None, op0=ALU.add), RS, RS)
                    vop(lambda e: e.reciprocal(out=w1, in_=dd), RS, RS)
                    vop(lambda e: e.tensor_tensor(out=w2, in0=ee, in1=w1, op=ALU.mult), RS, RS)
                    vop(lambda e: e.tensor_tensor(out=within, in0=oh1, in1=bc(w1), op=ALU.mult), RW + RS, RW)
                    vop(lambda e: e.tensor_tensor(out=tmp4, in0=oh2, in1=bc(w2), op=ALU.mult), RW + RS, RW)
                    vop(lambda e: e.tensor_tensor(out=within, in0=within, in1=tmp4, op=ALU.add), RW, RW)
                    vop(lambda e: e.tensor_tensor(out=gw, in0=goh, in1=bc(gp), op=ALU.mult), RW + RS, RW)
                    for g in range(4):
                        gwb = rw[:, 8, :, g:g + 1].broadcast_to([128, NTP, 4])
                        vop(lambda e, g=g, gwb=gwb: e.tensor_tensor(out=comb[:, :, 4 * g:4 * g + 4], in0=within, in1=gwb, op=ALU.mult),
                            RW, [b["comb"]])

                    first_pass = (seq == 0 and p == 0)
                    ecnt = [0]

                    def load_expert(ex, scratch=False):
                        sl = ex % 2
                        if scratch or not first_pass:
                            K.dma(SP, wgb[sl][:].rearrange("p k n -> p (k n)"), wsc_g[ex], None, reads=[b_wsc[ex]], writes=[b_wgb[sl]])
                            K.dma(SP, wubx[sl][:].rearrange("p k n -> p (k n)"), wsc_u[ex], None, reads=[b_wsc[ex]], writes=[b_wubx[sl]])
                            K.dma(SP, wdb[sl][:].rearrange("p k n -> p (k n)"), wsc_d[ex], None, reads=[b_wsc[ex]], writes=[b_wdb[sl]])
                            return
                        stg_t = [estg[0], estg[1], h2f, tmpf2, h2Tf[0], h2Tf[1]]
                        stg_b = [b_estg[0], b_estg[1], b["h2f"], b["tmpf2"], b_h2Tf[0], b_h2Tf[1]]

                        def stg_view(es, k):
                            tt_ = stg_t[es]
                            if es >= 4:
                                return tt_[:].rearrange("p k c -> p (k c)").rearrange("p (k n) -> p k n", k=k)
                            return tt_[:].rearrange("p (k n) -> p k n", k=k)

                        for (src, dstt, dbuf, ce) in ((w_gate, wgb, b_wgb, ACT), (w_up, wubx, b_wubx, POOL)):
                            for i in range(4):
                                es = ecnt[0] % 6
                                ecnt[0] += 1
                                K.dma(SP, stg_view(es, 2),
                                      src[ex, i * 256:(i + 1) * 256, :].rearrange("(k p) n -> p k n", p=128), None, writes=[stg_b[es]])
                                if ce is ACT:
                                    K.op(ce, lambda e, es=es, i=i, dstt=dstt, sl=sl: e.copy(
                                        out=dstt[sl][:, 2 * i:2 * i + 2, :], in_=stg_view(es, 2)),
                                        reads=[stg_b[es]], writes=[dbuf[sl]])
                                else:
                                    K.op(ce, lambda e, es=es, i=i, dstt=dstt, sl=sl: e.tensor_copy(
                                        out=dstt[sl][:, 2 * i:2 * i + 2, :], in_=stg_view(es, 2)),
                                        reads=[stg_b[es]], writes=[dbuf[sl]])
                        for i in range(4):
                            es = ecnt[0] % 6
                            ecnt[0] += 1
                            K.dma(SP, stg_view(es, 1), w_down[ex:ex + 1, i * 128:(i + 1) * 128, :].rearrange("o p n -> p o n"), None, writes=[stg_b[es]])
                            K.op(DVE, lambda e, es=es, i=i, sl=sl: e.tensor_copy(out=wdb[sl][:, i:i + 1, :], in_=stg_view(es, 1)),
                                 reads=[stg_b[es]], writes=[b_wdb[sl]])
                        K.dma(SP, wsc_g[ex], wgb[sl][:].rearrange("p k n -> p (k n)"), None, reads=[b_wgb[sl]], writes=[b_wsc[ex]])
                        K.dma(SP, wsc_u[ex], wubx[sl][:].rearrange("p k n -> p (k n)"), None, reads=[b_wubx[sl]], writes=[b_wsc[ex]])
                        K.dma(SP, wsc_d[ex], wdb[sl][:].rearrange("p k n -> p (k n)"), None, reads=[b_wdb[sl]], writes=[b_wsc[ex]])

                    blocks = [(ex, jb) for ex in range(NE) for jb in range(NBP)]
                    gcnt = [0]
                    ycnt = [0]

                    def stage_a(bi):
                        ex, jb = blocks[bi]
                        sl = ex % 2
                        cs = slice(jb * 512, (jb + 1) * 512)
                        for mch in range(4):
                            gi = gcnt[0] % 2
                            gcnt[0] += 1
                            gb, ub = gi, 2 + gi
                            ai = (bi % 2) * 4 + mch
                            for k in range(8):
                                K.op(PE, lambda e, k=k, sl=sl, mch=mch, gb=gb, cs=cs: e.matmul(
                                    bank(gb), lhsT=wgb[sl][:, k, mch * 128:(mch + 1) * 128], rhs=h2T[:, k, cs],
                                    start=(k == 0), stop=(k == 7)),
                                    reads=[b_wgb[sl], b["h2T"]], writes=[PB[gb]], inc=(k == 7))
                            for k in range(8):
                                K.op(PE, lambda e, k=k, sl=sl, mch=mch, ub=ub, cs=cs: e.matmul(
                                    bank(ub), lhsT=wubx[sl][:, k, mch * 128:(mch + 1) * 128], rhs=h2T[:, k, cs],
                                    start=(k == 0), stop=(k == 7)),
                                    reads=[b_wubx[sl], b["h2T"]], writes=[PB[ub]], inc=(k == 7))
                            K.op(ACT, lambda e, gi=gi, gb=gb: e.activation(out=sg[gi][:], in_=bank(gb), func=AF.Silu),
                                 reads=[PB[gb]], writes=[b_sg[gi]])
                            K.op(DVE, lambda e, gi=gi, ub=ub, ai=ai: e.tensor_tensor(out=aT[ai][:], in0=bank(ub), in1=sg[gi][:], op=ALU.mult),
                                 reads=[PB[ub], b_sg[gi]], writes=[b_aT[ai]])

                    def stage_b(bi):
                        ex, jb = blocks[bi]
                        sl = ex % 2
                        for t4 in range(4):
                            tt = jb * 4 + t4
                            t = p * NTP + tt
                            yi = ycnt[0] % 2
                            yp = 2 + yi
                            ycnt[0] += 1
                            for half in range(2):
                                for mch in range(4):
                                    ai = (bi % 2) * 4 + mch
                                    K.op(PE, lambda e, t4=t4, half=half, mch=mch, ai=ai, yp=yp, sl=sl: e.matmul(
                                        bank(2 * yp + half), lhsT=aT[ai][:, t4 * 128:(t4 + 1) * 128],
                                        rhs=wdb[sl][:, mch, half * 512:(half + 1) * 512], start=(mch == 0), stop=(mch == 3)),
                                        reads=[b_aT[ai], b_wdb[sl]], writes=[PB[2 * yp + half]], inc=(mch == 3))
                            K.op(DVE, lambda e, tt=tt, ex=ex, yp=yp, yi=yi: e.scalar_tensor_tensor(
                                out=ytmp[yi][:], in0=ps[yp][:], scalar=comb[:, tt, ex:ex + 1], in1=G2[:],
                                op0=ALU.mult, op1=ALU.mult),
                                reads=[PB[2 * yp], PB[2 * yp + 1], b["comb"], b["G2"]], writes=[b_ytmp[yi]])
                            K.op(POOL, lambda e, t=t, yi=yi: e.tensor_tensor(out=acc[:, t, :], in0=acc[:, t, :], in1=ytmp[yi][:], op=ALU.add),
                                 reads=[b_acc[t], b_ytmp[yi]], writes=[b_acc[t]])
                        if jb == NBP - 1 and ex + 2 < NE:
                            load_expert(ex + 2)

                    if p == 0:
                        load_expert(0)
                        load_expert(1)
                    for bi in range(len(blocks) + 1):
                        if bi < len(blocks):
                            stage_a(bi)
                        if bi >= 1:
                            stage_b(bi - 1)
                    if p + 1 < NPASS:
                        load_expert(0, scratch=True)
                        load_expert(1, scratch=True)

                    for tt in range(NTP):
                        t = p * NTP + tt
                        d4 = tt % 4
                        ssap = stat[:, 12 + d4:13 + d4]
                        K.op(ACT, lambda e: e.activation(out=junk2[:], in_=acc[:, t, :], func=AF.Square, accum_out=ssap),
                             reads=[b_acc[t]], writes=[b_st[4 + d4]])
                        rstd_from_ss(ssap, 1.0 / D, b_st[4 + d4])
                        K.op(DVE, lambda e: e.scalar_tensor_tensor(out=acc[:, t, :], in0=acc[:, t, :], scalar=ssap, in1=gfin_bc[:],
                                                                   op0=ALU.mult, op1=ALU.mult),
                             reads=[b_acc[t], b_st[4 + d4], b["gfin_bc"]], writes=[b_acc[t]])
                        K.dma(SP, out[seq, t * 128:(t + 1) * 128, :], acc[:, t, :], s_out, reads=[b_acc[t]])
                K.barrier(new_sems=(seq + 1 < NSEQ))


        except _Stop:
            K.barrier()
            if STOP == 'b2':
                dflat = out[0].rearrange("(p a) d -> p (a d)", p=128)
                for k in range(8):
                    for jj in range(S // 512):
                        K.op(DVE, lambda e, k=k, jj=jj: e.tensor_copy(out=tA[:], in_=oT[:, k, jj * 512:(jj + 1) * 512]),
                             reads=[b_oT[k]], writes=[b["tA"]])
                        K.dma(SP, dflat[:, k * S + jj * 512:k * S + (jj + 1) * 512], tA[:], s_out, reads=[b["tA"]])
            if STOP == 'b3':
                for t in range(NT):
                    K.dma(SP, out[0, t * 128:(t + 1) * 128, :], acc[:, t, :], s_out, reads=[b_acc[t]])
        K.finish([(SP, s_out)])
    return nc


def _consts(S):
    ident = np.eye(128, dtype=np.float32)
    rt = np.zeros((128, 128), dtype=np.float32)
    for p in range(128):
        if p % 64 < 32:
            rt[p + 32, p] = -1.0
        else:
            rt[p - 32, p] = 1.0
    kk = np.arange(128)[:, None]
    qq = np.arange(128)[None, :]
    tri = (kk <= qq).astype(np.float32)
    prev = (qq < kk).astype(np.float32)
    swam = np.concatenate([prev, tri], axis=1)
    inv = (10000.0 ** (-np.arange(0, 64, 2, dtype=np.float32) / np.float32(64))).astype(np.float32)
    ang = np.arange(S, dtype=np.float32)[:, None] * inv[None, :]
    ang = np.concatenate([ang, ang], axis=-1)
    cos = np.cos(ang).astype(np.float32).T
    sin = np.sin(ang).astype(np.float32).T
    cosT = np.concatenate([cos, cos], axis=0).astype(ml_dtypes.bfloat16)
    sinT = np.concatenate([sin, sin], axis=0).astype(ml_dtypes.bfloat16)
    return ident, rt, tri, swam, np.ascontiguousarray(cosT), np.ascontiguousarray(sinT)


_NC_CACHE = {}


def kernel(x, c, w_ada, b_ada, g_mix, w_in, diff_lambda, g_diff_sub, swa_sinks, w_out,
           g_ffn, w_route_group, b_route_group, w_route_expert, b_route_expert,
           w_gate, w_up, w_down, g_final):
    x = np.asarray(x, dtype=np.float32)
    B, S, _ = x.shape
    NSEQ = B // NCORES
    f = lambda a: np.ascontiguousarray(np.asarray(a, dtype=np.float32))
    w_in0 = f(w_in)[0]
    cols = []
    for h in range(4):
        cols.append(w_in0[:, h * 128:(h + 1) * 128])
        cols.append(w_in0[:, 512 + h * 128:512 + (h + 1) * 128])
    for ch in range(4):
        hk = ch // 2
        cols.append(w_in0[:, 1536 + ch * 128:1536 + (ch + 1) * 128])
        skh = w_in0[:, 2048 + hk * 64:2048 + (hk + 1) * 64]
        cols.append(skh)
        cols.append(skh)
    cols.append(w_in0[:, 1024:1536])
    cols.append(w_in0[:, 2176:2304])
    w_in_ext = np.ascontiguousarray(np.concatenate(cols, axis=1))
    assert w_in_ext.shape == (1024, 2688)
    sinks = f(swa_sinks)[0]
    sink_c = np.zeros((128, 4), np.float32)
    for ch in range(4):
        sink_c[0:64, ch] = sinks[2 * ch]
        sink_c[64:128, ch] = sinks[2 * ch + 1]
    ident, rt, tri, swam, cosT, sinT = _consts(S)
    w_r = np.ascontiguousarray(np.concatenate([f(w_route_group)[0], f(w_route_expert)[0]], axis=1))
    b_r = np.ascontiguousarray(np.concatenate([f(b_route_group)[0], f(b_route_expert)[0]])[None, :])
    shared = dict(
        w_ada=f(w_ada)[0], b_ada=f(b_ada), g_mix=f(g_mix), g_ffn=f(g_ffn), g_fin=f(g_final)[None, :],
        w_in=w_in_ext, dlam=f(diff_lambda).reshape(1, 256), gds_c=f(g_diff_sub).reshape(128, 1), sink_c=sink_c,
        w_out=f(w_out)[0], w_r=w_r, b_r=b_r, w_gate=f(w_gate)[0], w_up=f(w_up)[0], w_down=f(w_down)[0],
        c_ident=ident, c_rt=rt, c_tri=tri, c_swam=swam, c_cos=cosT, c_sin=sinT,
    )
    cf = f(c)
    in_maps = []
    for i in range(NCORES):
        m = dict(shared)
        m["x"] = np.ascontiguousarray(x[i * NSEQ:(i + 1) * NSEQ])
        cc = cf[i * NSEQ:(i + 1) * NSEQ]
        m["cT"] = np.ascontiguousarray(cc.reshape(NSEQ, 8, 128).transpose(2, 1, 0))
        in_maps.append(m)
    key = (NSEQ, S)
    if key not in _NC_CACHE:
        _NC_CACHE[key] = build_nc(NSEQ, S)
    nc = _NC_CACHE[key]
    res = run_bass_kernel_spmd(nc, in_maps, core_ids=list(range(NCORES)))
    outs = [np.asarray(r["out"], dtype=np.float32).reshape(NSEQ, S, D) for r in res.results]
    return np.concatenate(outs, axis=0)
```

```python
import math
import types
from contextlib import ExitStack

import numpy as np
import ml_dtypes

import concourse.bass as bass
import concourse.mybir as mybir
from concourse.bass_utils import run_bass_kernel_spmd

F32 = mybir.dt.float32
BF16 = mybir.dt.bfloat16
AF = mybir.ActivationFunctionType
ALU = mybir.AluOpType
AX = mybir.AxisListType

D = 1024
NCORES = 8
EPS = 1e-6
SB_BASE = 16512
SB_LIMIT = 229376
KB = 1024


def _freeze(fn):
    if fn.__closure__ is None:
        return fn
    cells = []
    for c in fn.__closure__:
        try:
            cells.append(types.CellType(c.cell_contents))
        except ValueError:
            cells.append(c)
    return types.FunctionType(fn.__code__, fn.__globals__, fn.__name__, fn.__defaults__, tuple(cells))


class Sem:
    def __init__(self, h):
        self.h = h
        self.v = 0


class Eng:
    def __init__(self, name, sem, is_pe=False):
        self.name = name
        self.sem = sem
        self.seen = {}
        self.q = []
        self.is_pe = is_pe


class Buf:
    __slots__ = ("name", "w", "r", "dsem")

    def __init__(self, name, reg):
        self.name = name
        self.w = {}
        self.r = {}
        self.dsem = None
        reg.append(self)


class Sched:
    def __init__(self, nc, stack):
        self.nc = nc
        self.stack = stack
        self.bufs = []
        self.dma_sems = []
        self.nsem = 0
        self.pe = Eng("pe", self.new_sem("pe"), is_pe=True)
        self.act = Eng("act", self.new_sem("act"))
        self.dve = Eng("dve", self.new_sem("dve"))
        self.pool = Eng("pool", self.new_sem("pool"))
        self.sp = Eng("sp", self.new_sem("sp"))
        self.engs = [self.pe, self.act, self.dve, self.pool, self.sp]

    def new_sem(self, name):
        self.nsem += 1
        h = self.stack.enter_context(self.nc.semaphore(f"s{self.nsem}_{name}"))
        return Sem(h)

    def new_dma_sem(self, name):
        s = self.new_sem(name)
        self.dma_sems.append(s)
        return s

    def buf(self, name):
        return Buf(name, self.bufs)

    def _waits(self, E, deps):
        for s, v in deps.items():
            if s is E.sem:
                if E.is_pe or v > s.v:
                    continue
            if E.seen.get(s, 0) >= v:
                continue
            assert v <= s.v, f"wait on pending count {E.name} {v} > {s.v}"
            E.q.append(lambda eng, h=s.h, v=v: eng.wait_ge(h, v))
            E.seen[s] = v

    @staticmethod
    def _deps(reads, writes):
        deps = {}
        for b in reads:
            for s, v in b.w.items():
                if v > deps.get(s, 0):
                    deps[s] = v
        for b in writes:
            for s, v in b.w.items():
                if v > deps.get(s, 0):
                    deps[s] = v
            for s, v in b.r.items():
                if v > deps.get(s, 0):
                    deps[s] = v
        return deps

    def op(self, E, fn, reads=(), writes=(), inc=True):
        fn = _freeze(fn)
        self._waits(E, self._deps(reads, writes))
        if inc:
            E.sem.v += 1
            val = E.sem.v
            E.q.append(lambda eng, fn=fn, h=E.sem.h: fn(eng).then_inc(h, 1))
        else:
            val = E.sem.v + 1
            E.q.append(lambda eng, fn=fn: fn(eng))
        s = E.sem
        for b in reads:
            if b.r.get(s, 0) < val:
                b.r[s] = val
        for b in writes:
            if b.w.get(s, 0) < val:
                b.w[s] = val

    def dma(self, Q, out, in_, sem, reads=(), writes=()):
        if sem is None:
            wb = writes[0]
            if wb.dsem is None:
                wb.dsem = self.new_dma_sem("d_" + wb.name)
            sem = wb.dsem
        self._waits(Q, self._deps(reads, writes))
        sem.v += 16
        Q.q.append(lambda eng, out=out, in_=in_, h=sem.h: eng.dma_start(out=out, in_=in_).then_inc(h, 16))
        for b in reads:
            b.r[sem] = sem.v
        for b in writes:
            b.w[sem] = sem.v

    def barrier(self, new_sems=False):
        allsems = [E.sem for E in self.engs[:4]] + self.dma_sems
        for E in self.engs:
            deps = {s: s.v for s in allsems if s.v > 0 and s is not E.sem}
            self._waits(E, deps)
        for b in self.bufs:
            b.w.clear()
            b.r.clear()
        if new_sems:
            for E in self.engs[:4]:
                E.sem = self.new_sem(E.name)

    def finish(self, final_waits):
        for E, s in final_waits:
            self._waits(E, {s: s.v})
        with self.nc.Block() as block:
            @block.tensor
            def _(e):
                for f in self.pe.q:
                    f(e)

            @block.scalar
            def _(e):
                for f in self.act.q:
                    f(e)

            @block.vector
            def _(e):
                for f in self.dve.q:
                    f(e)

            @block.gpsimd
            def _(e):
                for f in self.pool.q:
                    f(e)

            @block.sync
            def _(e):
                for f in self.sp.q:
                    f(e)


class Region:
    def __init__(self, nc, start, end, tag):
        self.nc = nc
        self.off = start
        self.end = end
        self.tag = tag

    def alloc(self, name, shape, dt):
        esz = 4 if dt == F32 else 2
        nbytes = int(np.prod(shape[1:])) * esz
        o = self.off
        self.off += (nbytes + 63) // 64 * 64
        assert self.off <= self.end, f"SBUF region {self.tag} overflow at {name}: {self.off} > {self.end}"
        return self.nc.alloc_sbuf_tensor_at(f"{self.tag}_{name}", shape, dt, offset=o)


class _Stop(Exception):
    pass


STOP = None


def build_nc(NSEQ, S):
    NT = S // 128
    NB = S // 512
    TP = min(S, 1024)
    NPASS = S // TP
    NTP = TP // 128
    NBP = TP // 512
    NE = 16

    nc = bass.Bass("TRN2", target_bir_lowering=False)

    def din(name, shape, dt=F32):
        return nc.dram_tensor(name, shape, dt, kind="ExternalInput").ap()

    x = din("x", [NSEQ, S, D])
    cT = din("cT", [128, 8, NSEQ])
    w_ada = din("w_ada", [D, 6 * D])
    b_ada = din("b_ada", [1, 6 * D])
    g_mix = din("g_mix", [1, D])
    g_ffn = din("g_ffn", [1, D])
    g_fin = din("g_fin", [1, D])
    w_in = din("w_in", [D, 2688])
    dlam = din("dlam", [1, 256])
    gds_c = din("gds_c", [128, 1])
    sink_c = din("sink_c", [128, 4])
    w_out = din("w_out", [D, D])
    w_r = din("w_r", [D, 20])
    b_r = din("b_r", [1, 20])
    w_gate = din("w_gate", [NE, D, 512])
    w_up = din("w_up", [NE, D, 512])
    w_down = din("w_down", [NE, 512, D])
    c_ident = din("c_ident", [128, 128])
    c_rt = din("c_rt", [128, 128])
    c_tri = din("c_tri", [128, 128])
    c_swam = din("c_swam", [128, 256])
    c_cos = din("c_cos", [128, S], BF16)
    c_sin = din("c_sin", [128, S], BF16)
    out = nc.dram_tensor("out", [NSEQ, S, D], F32, kind="ExternalOutput").ap()
    mod_scr = nc.dram_tensor("mod_scr", [NSEQ, 6 * D], F32).ap()
    wsc_g = nc.dram_tensor("wsc_g", [NE, 128, 4096], BF16).ap()
    wsc_u = nc.dram_tensor("wsc_u", [NE, 128, 4096], BF16).ap()
    wsc_d = nc.dram_tensor("wsc_d", [NE, 128, 4096], BF16).ap()

    with ExitStack() as st:
        K = Sched(nc, st)
        PE, ACT, DVE, POOL, SP = K.pe, K.act, K.dve, K.pool, K.sp

        P0 = SB_BASE
        OW0 = P0 + 29 * KB
        X0 = OW0 + 48 * KB
        rP = Region(nc, P0, OW0, "P")
        ident_f = rP.alloc("ident_f", [128, 128], F32)
        ident_b = rP.alloc("ident_b", [128, 128], BF16)
        rt_b = rP.alloc("rt_b", [128, 128], BF16)
        tri_b = rP.alloc("tri_b", [128, 128], BF16)
        swam_b = rP.alloc("swam_b", [128, 256], BF16)
        ones_b = rP.alloc("ones_b", [128, 128], BF16)
        ones_f = rP.alloc("ones_f", [128, 128], F32)
        gfin_bc = rP.alloc("gfin_bc", [128, D], F32)
        wr_f = rP.alloc("wr_f", [128, 8, 20], F32)
        br_f = rP.alloc("br_f", [128, 20], F32)
        gds = rP.alloc("gds", [128, 1], F32)
        neglam = rP.alloc("neglam", [128, 1], F32)
        esink = rP.alloc("esink", [128, 4], F32)
        epsc = rP.alloc("epsc", [128, 1], F32)
        cact = rP.alloc("cact", [128, 8, NSEQ], F32)
        lamw = rP.alloc("lamw", [128, 256], F32)
        lamt = rP.alloc("lamt", [128, 8], F32)
        stat = rP.alloc("stat", [128, 64], F32)
        A1 = rP.alloc("A1", [128, D], F32)
        B1 = rP.alloc("B1", [128, D], F32)
        A2 = rP.alloc("A2", [128, D], F32)
        B2 = rP.alloc("B2", [128, D], F32)
        G2 = rP.alloc("G2", [128, D], F32)

        rOW = Region(nc, OW0, X0, "OW")
        oT = rOW.alloc("oT", [128, 8, S], BF16)
        wob = rOW.alloc("wob", [128, 8, D], BF16)

        rS1 = Region(nc, X0, SB_LIMIT, "S1")
        hT = rS1.alloc("hT", [128, 8, S], BF16)
        xs = [rS1.alloc(f"xs{i}", [128, D], F32) for i in range(3)]
        tmpf = rS1.alloc("tmpf", [128, D], F32)
        tmpfb = rS1.alloc("tmpfb", [128, D], F32)
        hb = rS1.alloc("hb", [128, D], BF16)
        hbb = rS1.alloc("hbb", [128, D], BF16)
        junk = rS1.alloc("junk", [128, D], BF16)
        rPro = Region(nc, X0, SB_LIMIT, "Pro")
        adst = [rPro.alloc(f"adst{i}", [128, 8, 512], F32) for i in range(2)]
        bst = [rPro.alloc(f"bst{i}", [128, 512], F32) for i in range(2)]
        modsb = [rPro.alloc(f"modsb{i}", [128, 512], F32) for i in range(2)]
        cstg = rPro.alloc("cstg", [128, 256], F32)


        rS2 = Region(nc, X0, SB_LIMIT, "S2")
        rS2.off += 8 * S * 2
        hT2 = hT
        dv = rS2.alloc("dv", [128, NT, 512], BF16)
        svd = rS2.alloc("svd", [128, NT, 2, 128], BF16)
        wstg = [rS2.alloc(f"wstg{i}", [128, 8, 256], F32) for i in range(2)]
        wub = [rS2.alloc(f"wub{i}", [128, 8, 256], BF16) for i in range(2)]
        qT = rS2.alloc("qT", [128, S], BF16)
        kz = [rS2.alloc(f"kz{i}", [128, S], BF16) for i in range(2)]
        cosb = rS2.alloc("cosb", [128, S], BF16)
        sinb = rS2.alloc("sinb", [128, S], BF16)
        pT = [rS2.alloc(f"pT{i}", [128, 512], BF16) for i in range(4)]
        qraw_s = [rS2.alloc(f"qraw{i}", [128, 512], BF16) for i in range(3)]
        rt1_s = [rS2.alloc(f"rt1_{i}", [128, 512], F32) for i in range(3)]
        rt2_s = [rS2.alloc(f"rt2_{i}", [128, 512], F32) for i in range(3)]
        tA = rS2.alloc("tA", [128, 512], F32)
        tB = rS2.alloc("tB", [128, 512], F32)
        tC = rS2.alloc("tC", [128, 512], F32)
        tD = rS2.alloc("tD", [128, 512], F32)
        sqb = rS2.alloc("sqb", [128, 512], BF16)
        svsb = rS2.alloc("svsb", [128, 128], BF16)

        rM = Region(nc, X0, SB_LIMIT, "M")
        acc = rM.alloc("acc", [128, NT, D], F32)
        xs5 = [rM.alloc(f"xs5_{i}", [128, D], F32) for i in range(2)]
        wostg = [rM.alloc(f"wostg{i}", [128, 2, D], F32) for i in range(2)]
        g1t = rM.alloc("g1t", [128, D], F32)
        rMo = Region(nc, OW0, X0, "Mo")
        wgb = [rMo.alloc(f"wgb{i}", [128, 8, 512], BF16) for i in range(2)]
        wubx = [rMo.alloc(f"wubx{i}", [128, 8, 512], BF16) for i in range(2)]
        wdb = [rMo.alloc(f"wdb{i}", [128, 4, D], BF16) for i in range(2)]
        rM2 = Region(nc, X0 + NT * D * 4, SB_LIMIT, "M2")
        h2T = rM2.alloc("h2T", [128, 8, TP], BF16)
        h2f = rM2.alloc("h2f", [128, D], F32)
        tmpf2 = rM2.alloc("tmpf2", [128, D], F32)
        h2Tf = [rM2.alloc(f"h2Tf{i}", [128, 8, 128], F32) for i in range(2)]
        junk2 = rM2.alloc("junk2", [128, D], BF16)
        ytmp = [rM2.alloc(f"ytmp{i}", [128, D], F32) for i in range(2)]
        estg = [rM2.alloc(f"estg{i}", [128, 1024], F32) for i in range(2)]
        aT = [rM2.alloc(f"aT{i}", [128, 512], BF16) for i in range(8)]
        sg = [rM2.alloc(f"sg{i}", [128, 512], BF16) for i in range(2)]
        lg = rM2.alloc("lg", [128, NTP, 20], F32)
        comb = rM2.alloc("comb", [128, NTP, 16], F32)
        rw = rM2.alloc("rw", [128, 16, NTP, 4], F32)
        rs = rM2.alloc("rs", [128, 16, NTP], F32)

        ps = [st.enter_context(nc.psum_tensor(f"ps{i}", [128, 1024], F32)) for i in range(4)]
        PB = [K.buf(f"pb{i}") for i in range(8)]

        def bank(i):
            return ps[i // 2][:, (i % 2) * 512:(i % 2 + 1) * 512]

        def mk(*names):
            return {n: K.buf(n) for n in names}

        b = mk("ident_f", "ident_b", "rt_b", "tri_b", "swam_b", "ones_b", "ones_f", "gfin_bc", "wr_f", "br_f",
               "gds", "neglam", "esink", "cact", "lamw", "lamt", "stat", "A1", "B1", "A2", "B2", "G2",
               "wob", "hT", "tmpf", "hb", "junk", "cstg", "dv", "svd", "qT", "kz0", "kz1", "cosb", "sinb",
               "qraw", "rt1", "rt2", "tA", "tB", "tC", "tD", "epsc", "sqb", "svsb", "g1t", "h2T", "h2f", "tmpf2", "junk2",
               "lg", "comb", "rw", "rs", "mod_scr")
        b_xs = [K.buf(f"xs{i}") for i in range(3)]
        b_tmpf2s = [b["tmpf"], K.buf("tmpfb")]
        b_hb2s = [b["hb"], K.buf("hbb")]
        b_st = [K.buf(f"st{i}") for i in range(8)]
        b_adst = [K.buf(f"adst{i}") for i in range(2)]
        b_bst = [K.buf(f"bst{i}") for i in range(2)]
        b_modsb = [K.buf(f"modsb{i}") for i in range(2)]
        b_wstg = [K.buf(f"wstg{i}") for i in range(2)]
        b_qraw = [K.buf(f"qraw{i}") for i in range(3)]
        b_rt1 = [K.buf(f"rt1_{i}") for i in range(3)]
        b_rt2 = [K.buf(f"rt2_{i}") for i in range(3)]
        b_wub = [K.buf(f"wub{i}") for i in range(2)]
        b_pT = [K.buf(f"pT{i}") for i in range(4)]
        b_oT = [K.buf(f"oT{i}") for i in range(8)]
        b_acc = [K.buf(f"acc{i}") for i in range(NT)]
        b_xs5 = [K.buf(f"xs5_{i}") for i in range(2)]
        b_wostg = [K.buf(f"wostg{i}") for i in range(2)]
        b_wgb = [K.buf(f"wgb{i}") for i in range(2)]
        b_wubx = [K.buf(f"wubx{i}") for i in range(2)]
        b_wdb = [K.buf(f"wdb{i}") for i in range(2)]
        b_h2Tf = [K.buf(f"h2Tf{i}") for i in range(2)]
        b_ytmp = [K.buf(f"ytmp{i}") for i in range(2)]
        b_estg = [K.buf(f"estg{i}") for i in range(2)]
        b_wsc = [K.buf(f"wsc{i}") for i in range(NE)]
        b_aT = [K.buf(f"aT{i}") for i in range(8)]
        b_sg = [K.buf(f"sg{i}") for i in range(2)]

        s_const = K.new_dma_sem("const")
        s_x = [K.new_dma_sem(f"x{i}") for i in range(3)]
        s_w = [K.new_dma_sem(f"w{i}") for i in range(2)]
        s_mod = K.new_dma_sem("mod")
        s_misc = K.new_dma_sem("misc")
        s_out = K.new_dma_sem("out")

        def _ck(name):
            if STOP == name:
                raise _Stop()

        try:
            def load_const_cast(dst, dst_buf, src, width):
                K.dma(SP, cstg[:, 0:width], src, None, writes=[b["cstg"]])
                K.op(DVE, lambda e: e.tensor_copy(out=dst[:], in_=cstg[:, 0:width]), reads=[b["cstg"]], writes=[dst_buf])

            K.dma(SP, ident_f[:], c_ident, None, writes=[b["ident_f"]])
            K.op(DVE, lambda e: e.tensor_copy(out=ident_b[:], in_=ident_f[:]), reads=[b["ident_f"]], writes=[b["ident_b"]])
            load_const_cast(rt_b, b["rt_b"], c_rt, 128)
            load_const_cast(tri_b, b["tri_b"], c_tri, 128)
            load_const_cast(swam_b, b["swam_b"], c_swam, 256)
            K.op(POOL, lambda e: e.memset(ones_b[:], 1.0), writes=[b["ones_b"]])
            K.op(POOL, lambda e: e.memset(ones_f[:], 1.0), writes=[b["ones_f"]])
            K.op(POOL, lambda e: e.memset(epsc[:], EPS), writes=[b["epsc"]])
            K.dma(SP, gfin_bc[:], g_fin.partition_broadcast(128), None, writes=[b["gfin_bc"]])
            K.dma(SP, wr_f[:], w_r.rearrange("(k p) n -> p k n", p=128), None, writes=[b["wr_f"]])
            K.dma(SP, br_f[:], b_r.partition_broadcast(128), None, writes=[b["br_f"]])
            K.dma(SP, gds[:], gds_c, None, writes=[b["gds"]])
            K.dma(SP, esink[:], sink_c, None, writes=[b["esink"]])
            K.dma(SP, cact[:], cT, None, writes=[b["cact"]])
            K.dma(SP, lamw[:], dlam.partition_broadcast(128), None, writes=[b["lamw"]])

            K.op(DVE, lambda e: e.tensor_scalar(out=gds[:], in0=gds[:], scalar1=0.8, scalar2=None, op0=ALU.mult),
                 reads=[b["gds"]], writes=[b["gds"]])
            K.op(ACT, lambda e: e.activation(out=esink[:], in_=esink[:], func=AF.Exp), reads=[b["esink"]], writes=[b["esink"]])
            K.op(DVE, lambda e: e.tensor_tensor(out=lamw[:, 0:64], in0=lamw[:, 0:64], in1=lamw[:, 64:128], op=ALU.mult),
                 reads=[b["lamw"]], writes=[b["lamw"]])
            K.op(DVE, lambda e: e.tensor_tensor(out=lamw[:, 128:192], in0=lamw[:, 128:192], in1=lamw[:, 192:256], op=ALU.mult),
                 reads=[b["lamw"]], writes=[b["lamw"]])
            K.op(DVE, lambda e: e.reduce_sum(out=lamt[:, 0:1], in_=lamw[:, 0:64], axis=AX.X), reads=[b["lamw"]], writes=[b["lamt"]])
            K.op(DVE, lambda e: e.reduce_sum(out=lamt[:, 1:2], in_=lamw[:, 128:192], axis=AX.X), reads=[b["lamw"]], writes=[b["lamt"]])
            K.op(ACT, lambda e: e.activation(out=lamt[:, 2:4], in_=lamt[:, 0:2], func=AF.Exp), reads=[b["lamt"]], writes=[b["lamt"]])
            K.op(DVE, lambda e: e.scalar_tensor_tensor(out=neglam[:], in0=lamt[:, 3:4], scalar=-0.2, in1=lamt[:, 2:3],
                                                       op0=ALU.add, op1=ALU.subtract),
                 reads=[b["lamt"]], writes=[b["neglam"]])
            K.op(ACT, lambda e: e.activation(out=cact[:], in_=cact[:], func=AF.Silu), reads=[b["cact"]], writes=[b["cact"]])

            for n in range(12):
                sl = n % 2
                K.dma(SP, adst[sl][:], w_ada[:, n * 512:(n + 1) * 512].rearrange("(k p) n -> p k n", p=128), None,
                      writes=[b_adst[sl]])
                K.dma(SP, bst[sl][0:NSEQ, :], b_ada[:, n * 512:(n + 1) * 512].partition_broadcast(NSEQ), None, writes=[b_bst[sl]])
                pb = n % 2
                for k in range(8):
                    K.op(PE, lambda e, k=k, sl=sl, pb=pb: e.matmul(bank(pb)[0:NSEQ, :], lhsT=cact[:, k, :], rhs=adst[sl][:, k, :],
                                                                   start=(k == 0), stop=(k == 7)),
                         reads=[b["cact"], b_adst[sl]], writes=[PB[pb]], inc=(k == 7))
                K.op(DVE, lambda e, sl=sl, pb=pb: e.tensor_tensor(out=modsb[sl][0:NSEQ, :], in0=bank(pb)[0:NSEQ, :], in1=bst[sl][0:NSEQ, :], op=ALU.add),
                     reads=[PB[pb], b_bst[sl]], writes=[b_modsb[sl]])
                K.dma(SP, mod_scr[:, n * 512:(n + 1) * 512], modsb[sl][0:NSEQ, :], s_mod, reads=[b_modsb[sl]], writes=[b["mod_scr"]])
            K.barrier()
            _ck('pro')

            def rstd_from_ss(ss_ap, scale, sbuf=None):
                sbuf = sbuf if sbuf is not None else b["stat"]
                K.op(ACT, lambda e: e.activation(out=ss_ap, in_=ss_ap, func=AF.Sqrt, scale=scale, bias=EPS),
                     reads=[sbuf], writes=[sbuf])
                K.op(DVE, lambda e: e.reciprocal(out=ss_ap, in_=ss_ap), reads=[sbuf], writes=[sbuf])

            def load_mod_vec(dst, dst_buf, seq, v, sem=None):
                K.dma(SP, dst[:], mod_scr[seq:seq + 1, v * D:(v + 1) * D].partition_broadcast(128), None,
                      reads=[b["mod_scr"]], writes=[dst_buf])

            for seq in range(NSEQ):
                load_mod_vec(B1, b["B1"], seq, 0)
                load_mod_vec(A1, b["A1"], seq, 1)
                K.dma(SP, tmpf[:], g_mix.partition_broadcast(128), None, writes=[b["tmpf"]])
                K.op(DVE, lambda e: e.scalar_tensor_tensor(out=A1[:], in0=A1[:], scalar=1.0, in1=tmpf[:], op0=ALU.add, op1=ALU.mult),
                     reads=[b["A1"], b["tmpf"]], writes=[b["A1"]])

                tmpf_s = [tmpf, tmpfb]
                hb_s = [hb, hbb]

                def s1_stage1(t):
                    sl = t % 3
                    d2 = t % 2
                    K.dma(SP, xs[sl][:], x[seq, t * 128:(t + 1) * 128, :], None, writes=[b_xs[sl]])
                    ssap = stat[:, 8 + d2:9 + d2]
                    K.op(ACT, lambda e: e.activation(out=junk[:], in_=xs[sl][:], func=AF.Square, accum_out=ssap),
                         reads=[b_xs[sl]], writes=[b_st[d2]])
                    rstd_from_ss(ssap, 1.0 / D, b_st[d2])
                    K.op(DVE, lambda e: e.scalar_tensor_tensor(out=tmpf_s[d2][:], in0=xs[sl][:], scalar=ssap, in1=A1[:],
                                                               op0=ALU.mult, op1=ALU.mult),
                         reads=[b_xs[sl], b_st[d2], b["A1"]], writes=[b_tmpf2s[d2]])
                    K.op(POOL, lambda e: e.tensor_tensor(out=hb_s[d2][:], in0=tmpf_s[d2][:], in1=B1[:], op=ALU.add),
                         reads=[b_tmpf2s[d2], b["B1"]], writes=[b_hb2s[d2]])

                def s1_stage2(t):
                    d2 = t % 2
                    pbi = t % 2
                    pbv = bank(pbi).bitcast(BF16)
                    for k in range(8):
                        K.op(PE, lambda e, k=k: e.transpose(out=pbv[:, k * 128:(k + 1) * 128], in_=hb_s[d2][:, k * 128:(k + 1) * 128],
                                                            identity=ident_b[:]),
                             reads=[b_hb2s[d2], b["ident_b"]], writes=[PB[pbi]], inc=(k == 7))
                    K.op(ACT, lambda e: e.copy(out=hT[:, :, t * 128:(t + 1) * 128], in_=pbv.rearrange("p (k c) -> p k c", k=8)),
                         reads=[PB[pbi]], writes=[b["hT"]])

                for t in range(NT + 1):
                    if t < NT:
                        s1_stage1(t)
                    if t >= 1:
                        s1_stage2(t - 1)
                K.barrier()
                _ck('b1')

                K.dma(SP, cosb[:], c_cos, None, writes=[b["cosb"]])
                K.dma(SP, sinb[:], c_sin, None, writes=[b["sinb"]])
                K.op(POOL, lambda e: e.memset(kz[0][:], 0.0), writes=[b["kz0"]])
                K.op(POOL, lambda e: e.memset(kz[1][:], 0.0), writes=[b["kz1"]])

                wcnt = [0]

                def load_wunit(col0, ncols):
                    sl = wcnt[0] % 2
                    wcnt[0] += 1
                    K.dma(SP, wstg[sl][:, :, 0:ncols], w_in[:, col0:col0 + ncols].rearrange("(k p) n -> p k n", p=128), None,
                          writes=[b_wstg[sl]])
                    K.op(POOL, lambda e, sl=sl: e.tensor_copy(out=wub[sl][:, :, 0:ncols], in_=wstg[sl][:, :, 0:ncols]),
                         reads=[b_wstg[sl]], writes=[b_wub[sl]])
                    return sl

                for piece, (c0, ncol) in enumerate([(2048, 256), (2304, 256), (2560, 128)]):
                    sl = load_wunit(c0, ncol)
                    for t in range(NT):
                        pbi = t % 2
                        for k in range(8):
                            K.op(PE, lambda e, k=k, t=t, sl=sl, pbi=pbi, ncol=ncol: e.matmul(
                                bank(pbi)[:, 0:ncol], lhsT=hT2[:, k, t * 128:(t + 1) * 128], rhs=wub[sl][:, k, 0:ncol],
                                start=(k == 0), stop=(k == 7)),
                                reads=[b["hT"], b_wub[sl]], writes=[PB[pbi]], inc=(k == 7))
                        if piece < 2:
                            K.op(ACT, lambda e, t=t, pbi=pbi, piece=piece: e.copy(out=dv[:, t, piece * 256:(piece + 1) * 256],
                                                                                  in_=bank(pbi)[:, 0:256]),
                                 reads=[PB[pbi]], writes=[b["dv"]])
                        else:
                            K.op(ACT, lambda e, pbi=pbi: e.copy(out=svsb[:], in_=bank(pbi)[:, 0:128]), reads=[PB[pbi]], writes=[b["svsb"]])
                            for hk in range(2):
                                for dup in range(2):
                                    K.op(POOL, lambda e, t=t, hk=hk, dup=dup: e.tensor_copy(
                                        out=svd[:, t, hk, dup * 64:(dup + 1) * 64], in_=svsb[:, hk * 64:(hk + 1) * 64]),
                                        reads=[b["svsb"]], writes=[b["svd"]])

                _ck('v')
                sl_next = load_wunit(0, 256)
                for u in range(8):
                    sl = sl_next
                    def rope_a(ridx):
                        j, which = divmod(ridx, 2)
                        cs = slice(j * 512, (j + 1) * 512)
                        pbi = (0, 1, 2, 7)[ridx % 4]
                        rsl = ridx % 3
                        for k in range(8):
                            K.op(PE, lambda e, k=k: e.matmul(
                                bank(pbi), lhsT=wub[sl][:, k, which * 128:(which + 1) * 128], rhs=hT2[:, k, cs],
                                start=(k == 0), stop=(k == 7)),
                                reads=[b["hT"], b_wub[sl]], writes=[PB[pbi]], inc=(k == 7))
                        qraw = qraw_s[rsl]
                        K.op(ACT, lambda e: e.copy(out=qraw[:], in_=bank(pbi)), reads=[PB[pbi]], writes=[b_qraw[rsl], PB[pbi]])

                    def rope_b(ridx):
                        j, which = divmod(ridx, 2)
                        cs = slice(j * 512, (j + 1) * 512)
                        pbi = (0, 1, 2, 7)[ridx % 4]
                        rb = (3, 4, 5, 6)[ridx % 4]
                        rsl = ridx % 3
                        qraw, rt1, rt2 = qraw_s[rsl], rt1_s[rsl], rt2_s[rsl]
                        bq, b1_, b2_ = b_qraw[rsl], b_rt1[rsl], b_rt2[rsl]
                        K.op(PE, lambda e: e.matmul(bank(rb), lhsT=rt_b[:], rhs=qraw[:], start=True, stop=True),
                             reads=[b["rt_b"], bq], writes=[PB[rb]])
                        K.op(DVE, lambda e: e.tensor_tensor(out=rt1[:], in0=bank(pbi), in1=cosb[:, cs], op=ALU.mult),
                             reads=[PB[pbi], b["cosb"]], writes=[b1_, PB[pbi]])
                        K.op(DVE, lambda e: e.tensor_tensor(out=rt2[:], in0=bank(rb), in1=sinb[:, cs], op=ALU.mult),
                             reads=[PB[rb], b["sinb"]], writes=[b2_])
                        if which == 0:
                            K.op(POOL, lambda e: e.tensor_tensor(out=qT[:, cs], in0=rt1[:], in1=rt2[:], op=ALU.add),
                                 reads=[b1_, b2_], writes=[b["qT"]])
                        else:
                            K.op(DVE, lambda e: e.tensor_tensor(out=kz[0][0:64, cs], in0=rt1[0:64, :], in1=rt2[0:64, :], op=ALU.add),
                                 reads=[b1_, b2_], writes=[b["kz0"]])
                            K.op(DVE, lambda e: e.tensor_tensor(out=kz[1][64:128, cs], in0=rt1[64:128, :], in1=rt2[64:128, :], op=ALU.add),
                                 reads=[b1_, b2_], writes=[b["kz1"]])

                    nrope = 2 * NB
                    for ridx in range(nrope + 1):
                        if ridx < nrope:
                            rope_a(ridx)
                        if ridx >= 1:
                            rope_b(ridx - 1)

                    _ck(f'proj{u}')
                    if u < 7:
                        sl_next = load_wunit((u + 1) * 256, 256)
                    if u < 4:
                        h = u
                        for j in range(NB):
                            items = [(c, m) for c in range(4 * j + 4) for m in range(2)]
                            nit = len(items)
                            last_c = 4 * j + 3

                            def s_stage(idx):
                                c, m = items[idx]
                                i = c - 4 * j
                                q0 = max(i, 0) * 128
                                sb_i = (0, 1, 2, 7)[idx % 4]
                                pt_i = idx % 4
                                K.op(PE, lambda e, c=c, m=m, q0=q0, sb_i=sb_i: e.matmul(
                                    bank(sb_i)[:, q0:512], lhsT=kz[m][:, c * 128:(c + 1) * 128], rhs=qT[:, j * 512 + q0:(j + 1) * 512],
                                    start=True, stop=True),
                                    reads=[b["kz0"], b["kz1"], b["qT"]], writes=[PB[sb_i]])
                                K.op(ACT, lambda e, q0=q0, sb_i=sb_i, pt_i=pt_i: e.activation(
                                    out=pT[pt_i][:, q0:512], in_=bank(sb_i)[:, q0:512], func=AF.Exp, scale=0.125),
                                    reads=[PB[sb_i]], writes=[b_pT[pt_i]])
                                if i >= 0:
                                    K.op(DVE, lambda e, q0=q0, pt_i=pt_i: e.tensor_tensor(
                                        out=pT[pt_i][:, q0:q0 + 128], in0=pT[pt_i][:, q0:q0 + 128], in1=tri_b[:], op=ALU.mult),
                                        reads=[b_pT[pt_i], b["tri_b"]], writes=[b_pT[pt_i]])

                            def pv_stage(idx):
                                c, m = items[idx]
                                i = c - 4 * j
                                q0 = max(i, 0) * 128
                                pt_i = idx % 4
                                ob = 3 + m
                                lb = 5 + m
                                K.op(PE, lambda e, c=c, q0=q0, pt_i=pt_i, ob=ob: e.matmul(
                                    bank(ob)[:, q0:512], lhsT=dv[:, c, h * 128:(h + 1) * 128], rhs=pT[pt_i][:, q0:512],
                                    start=(c == 0), stop=(c == last_c)),
                                    reads=[b["dv"], b_pT[pt_i]], writes=[PB[ob]], inc=False)
                                K.op(PE, lambda e, c=c, q0=q0, pt_i=pt_i, lb=lb: e.matmul(
                                    bank(lb)[:, q0:512], lhsT=ones_b[:], rhs=pT[pt_i][:, q0:512],
                                    start=(c == 0), stop=(c == last_c)),
                                    reads=[b["ones_b"], b_pT[pt_i]], writes=[PB[lb]])

                            for idx in range(nit + 3):
                                if idx < nit:
                                    s_stage(idx)
                                if idx >= 3:
                                    pv_stage(idx - 3)
                            cs = slice(j * 512, (j + 1) * 512)
                            K.op(ACT, lambda e: e.activation(out=tA[:], in_=bank(5), func=AF.Ln), reads=[PB[5]], writes=[b["tA"]])
                            K.op(DVE, lambda e: e.tensor_copy(out=tB[:], in_=bank(3)), reads=[PB[3]], writes=[b["tB"]])
                            K.op(ACT, lambda e: e.activation(out=tD[:], in_=bank(6), func=AF.Ln), reads=[PB[6]], writes=[b["tD"]])
                            K.op(DVE, lambda e: e.tensor_copy(out=tC[:], in_=bank(4)), reads=[PB[4]], writes=[b["tC"]])
                            K.op(ACT, lambda e: e.activation(out=tA[:], in_=tA[:], func=AF.Exp, scale=-1.0), reads=[b["tA"]], writes=[b["tA"]])
                            K.op(ACT, lambda e: e.activation(out=tD[:], in_=tD[:], func=AF.Exp, scale=-1.0), reads=[b["tD"]], writes=[b["tD"]])
                            K.op(DVE, lambda e: e.tensor_tensor(out=tB[:], in0=tB[:], in1=tA[:], op=ALU.mult),
                                 reads=[b["tB"], b["tA"]], writes=[b["tB"]])
                            K.op(DVE, lambda e: e.tensor_tensor(out=tC[:], in0=tC[:], in1=tD[:], op=ALU.mult),
                                 reads=[b["tC"], b["tD"]], writes=[b["tC"]])
                            K.op(DVE, lambda e: e.scalar_tensor_tensor(out=tB[:], in0=tC[:], scalar=neglam[:, 0:1], in1=tB[:],
                                                                       op0=ALU.mult, op1=ALU.add),
                                 reads=[b["tC"], b["tB"], b["neglam"]], writes=[b["tB"]])
                            K.op(ACT, lambda e: e.activation(out=sqb[:], in_=tB[:], func=AF.Square), reads=[b["tB"]], writes=[b["sqb"]])
                            K.op(PE, lambda e: e.matmul(bank(7), lhsT=ones_b[:], rhs=sqb[:], start=True, stop=True),
                                 reads=[b["ones_b"], b["sqb"]], writes=[PB[7]])
                            K.op(ACT, lambda e: e.activation(out=tA[:], in_=bank(7), func=AF.Ln, scale=1.0 / 128, bias=epsc[:, 0:1]),
                                 reads=[PB[7], b["epsc"]], writes=[b["tA"]])
                            K.op(ACT, lambda e: e.activation(out=tA[:], in_=tA[:], func=AF.Exp, scale=-0.5), reads=[b["tA"]], writes=[b["tA"]])
                            K.op(DVE, lambda e, cs=cs: e.scalar_tensor_tensor(out=oT[:, h, cs], in0=tB[:], scalar=gds[:, 0:1], in1=tA[:],
                                                                              op0=ALU.mult, op1=ALU.mult),
                                 reads=[b["tB"], b["gds"], b["tA"]], writes=[b_oT[h]])
                    else:
                        ch = u - 4
                        hk = ch // 2
                        for j in range(NB):
                            items = list(range(4 * j, 4 * j + 4))
                            nit = len(items)

                            def s_stage(idx):
                                t = items[idx]
                                sb_i = (0, 1, 2, 7)[idx % 4]
                                pt_i = idx % 4
                                lo = 0 if t > 0 else 128
                                for s in range(2):
                                    c0 = s * 256
                                    if t > 0:
                                        K.op(PE, lambda e, c0=c0, s=s: e.matmul(
                                            bank(sb_i)[:, c0:c0 + 128], lhsT=kz[s][:, (t - 1) * 128:t * 128], rhs=qT[:, t * 128:(t + 1) * 128],
                                            start=True, stop=True),
                                            reads=[b["kz0"], b["kz1"], b["qT"]], writes=[PB[sb_i]], inc=False)
                                    K.op(PE, lambda e, c0=c0, s=s: e.matmul(
                                        bank(sb_i)[:, c0 + 128:c0 + 256], lhsT=kz[s][:, t * 128:(t + 1) * 128], rhs=qT[:, t * 128:(t + 1) * 128],
                                        start=True, stop=True),
                                        reads=[b["kz0"], b["kz1"], b["qT"]], writes=[PB[sb_i]], inc=(s == 1))
                                src3 = bank(sb_i).rearrange("p (s c) -> p s c", s=2)[:, :, lo:256]
                                dst3 = pT[pt_i][:].rearrange("p (s c) -> p s c", s=2)[:, :, lo:256]
                                msk3 = swam_b[:, lo:256].unsqueeze(1).broadcast_to([128, 2, 256 - lo])
                                K.op(ACT, lambda e: e.activation(out=dst3, in_=src3, func=AF.Exp, scale=0.125),
                                     reads=[PB[sb_i]], writes=[b_pT[pt_i]])
                                K.op(DVE, lambda e: e.tensor_tensor(out=dst3, in0=dst3, in1=msk3, op=ALU.mult),
                                     reads=[b_pT[pt_i], b["swam_b"]], writes=[b_pT[pt_i]])

                            def pv_stage(idx):
                                t = items[idx]
                                pt_i = idx % 4
                                tq = t - 4 * j
                                reg = slice(tq * 128, (tq + 1) * 128)
                                for s in range(2):
                                    c0 = s * 256
                                    ob = 3 + s
                                    lb = 5 + s
                                    first = True
                                    if t > 0:
                                        K.op(PE, lambda e, c0=c0, ob=ob: e.matmul(
                                            bank(ob)[:, reg], lhsT=svd[:, t - 1, hk, :], rhs=pT[pt_i][:, c0:c0 + 128], start=True, stop=False),
                                            reads=[b["svd"], b_pT[pt_i]], writes=[PB[ob]], inc=False)
                                        first = False
                                    K.op(PE, lambda e, c0=c0, ob=ob, first=first: e.matmul(
                                        bank(ob)[:, reg], lhsT=svd[:, t, hk, :], rhs=pT[pt_i][:, c0 + 128:c0 + 256], start=first, stop=True),
                                        reads=[b["svd"], b_pT[pt_i]], writes=[PB[ob]], inc=False)
                                    if t > 0:
                                        K.op(PE, lambda e, c0=c0, lb=lb: e.matmul(
                                            bank(lb)[:, reg], lhsT=ones_b[:], rhs=pT[pt_i][:, c0:c0 + 128], start=True, stop=False),
                                            reads=[b["ones_b"], b_pT[pt_i]], writes=[PB[lb]], inc=False)
                                    K.op(PE, lambda e, c0=c0, lb=lb, first=first: e.matmul(
                                        bank(lb)[:, reg], lhsT=ones_b[:], rhs=pT[pt_i][:, c0 + 128:c0 + 256], start=first, stop=True),
                                        reads=[b["ones_b"], b_pT[pt_i]], writes=[PB[lb]], inc=(s == 1))

                            for idx in range(nit + 3):
                                if idx < nit:
                                    s_stage(idx)
                                if idx >= 3:
                                    pv_stage(idx - 3)
                            cs = slice(j * 512, (j + 1) * 512)
                            for s in range(2):
                                pr = slice(s * 64, (s + 1) * 64)
                                K.op(DVE, lambda e, s=s, pr=pr: e.tensor_scalar(out=tA[pr, :], in0=bank(5 + s)[pr, :], scalar1=esink[pr, ch:ch + 1],
                                                                                scalar2=None, op0=ALU.add),
                                     reads=[PB[5 + s], b["esink"]], writes=[b["tA"]])
                                K.op(ACT, lambda e, pr=pr: e.activation(out=tA[pr, :], in_=tA[pr, :], func=AF.Ln), reads=[b["tA"]], writes=[b["tA"]])
                                K.op(ACT, lambda e, pr=pr: e.activation(out=tA[pr, :], in_=tA[pr, :], func=AF.Exp, scale=-1.0), reads=[b["tA"]], writes=[b["tA"]])
                                K.op(DVE, lambda e, s=s, pr=pr, cs=cs: e.tensor_tensor(out=oT[pr, 4 + ch, cs], in0=bank(3 + s)[pr, :], in1=tA[pr, :],
                                                                                       op=ALU.mult),
                                     reads=[PB[3 + s], b["tA"]], writes=[b_oT[4 + ch]])
                    _ck(f'unit{u}')
                K.barrier()
                _ck('b2')

                load_mod_vec(g1t, b["g1t"], seq, 2)
                for i in range(4):
                    sl = i % 2
                    K.dma(SP, wostg[sl][:], w_out[i * 256:(i + 1) * 256, :].rearrange("(k p) n -> p k n", p=128), None,
                          writes=[b_wostg[sl]])
                    for kk in range(2):
                        K.op(DVE, lambda e, i=i, kk=kk, sl=sl: e.tensor_tensor(out=wob[:, 2 * i + kk, :], in0=wostg[sl][:, kk, :], in1=g1t[:],
                                                                               op=ALU.mult),
                             reads=[b_wostg[sl], b["g1t"]], writes=[b["wob"]])
                for t in range(NT):
                    sl = t % 2
                    K.dma(SP, xs5[sl][:], x[seq, t * 128:(t + 1) * 128, :], None, writes=[b_xs5[sl]])
                    pp = t % 2
                    for half in range(2):
                        for kk in range(8):
                            K.op(PE, lambda e, t=t, kk=kk, half=half, pp=pp: e.matmul(
                                bank(2 * pp + half), lhsT=oT[:, kk, t * 128:(t + 1) * 128], rhs=wob[:, kk, half * 512:(half + 1) * 512],
                                start=(kk == 0), stop=(kk == 7)),
                                reads=[b_oT[kk], b["wob"]], writes=[PB[2 * pp + half]], inc=(kk == 7))
                    K.op(DVE, lambda e, t=t, sl=sl, pp=pp: e.tensor_tensor(out=acc[:, t, :], in0=ps[pp][:], in1=xs5[sl][:], op=ALU.add),
                         reads=[PB[2 * pp], PB[2 * pp + 1], b_xs5[sl]], writes=[b_acc[t]])
                K.barrier()
                _ck('b3')

                load_mod_vec(B2, b["B2"], seq, 3)
                load_mod_vec(A2, b["A2"], seq, 4)
                load_mod_vec(G2, b["G2"], seq, 5)
                K.dma(SP, tmpf2[:], g_ffn.partition_broadcast(128), None, writes=[b["tmpf2"]])
                K.op(DVE, lambda e: e.scalar_tensor_tensor(out=A2[:], in0=A2[:], scalar=1.0, in1=tmpf2[:], op0=ALU.add, op1=ALU.mult),
                     reads=[b["A2"], b["tmpf2"]], writes=[b["A2"]])

                for p in range(NPASS):
                    tmpX = [tmpf2, ytmp[0]]
                    b_tmpX = [b["tmpf2"], b_ytmp[0]]
                    h2X = [h2f, ytmp[1]]
                    b_h2X = [b["h2f"], b_ytmp[1]]

                    def s6_st1(tt):
                        t = p * NTP + tt
                        d2 = tt % 2
                        ssap = stat[:, 10 + d2:11 + d2]
                        K.op(ACT, lambda e: e.activation(out=junk2[:], in_=acc[:, t, :], func=AF.Square, accum_out=ssap),
                             reads=[b_acc[t]], writes=[b_st[2 + d2]])
                        rstd_from_ss(ssap, 1.0 / D, b_st[2 + d2])
                        K.op(DVE, lambda e: e.scalar_tensor_tensor(out=tmpX[d2][:], in0=acc[:, t, :], scalar=ssap, in1=A2[:],
                                                                   op0=ALU.mult, op1=ALU.mult),
                             reads=[b_acc[t], b_st[2 + d2], b["A2"]], writes=[b_tmpX[d2]])
                        K.op(POOL, lambda e: e.tensor_tensor(out=h2X[d2][:], in0=tmpX[d2][:], in1=B2[:], op=ALU.add),
                             reads=[b_tmpX[d2], b["B2"]], writes=[b_h2X[d2]])

                    def s6_st2(tt):
                        d2 = tt % 2
                        pp = tt % 2
                        fs = tt % 2
                        for k in range(8):
                            K.op(PE, lambda e, k=k: e.transpose(out=ps[pp][:, k * 128:(k + 1) * 128], in_=h2X[d2][:, k * 128:(k + 1) * 128],
                                                                identity=ident_f[:]),
                                 reads=[b_h2X[d2], b["ident_f"]], writes=[PB[2 * pp], PB[2 * pp + 1]], inc=(k == 7))
                        K.op(DVE, lambda e: e.tensor_copy(out=h2Tf[fs][:], in_=ps[pp][:].rearrange("p (k c) -> p k c", k=8)),
                             reads=[PB[2 * pp], PB[2 * pp + 1]], writes=[b_h2Tf[fs]])
                        K.op(ACT, lambda e: e.copy(out=h2T[:, :, tt * 128:(tt + 1) * 128], in_=h2Tf[fs][:]),
                             reads=[b_h2Tf[fs]], writes=[b["h2T"]])

                    def s6_st3(tt):
                        fs = tt % 2
                        lb = 4 + tt % 2
                        for k in range(8):
                            K.op(PE, lambda e, k=k: e.matmul(bank(lb)[:, 0:20], lhsT=h2Tf[fs][:, k, :], rhs=wr_f[:, k, :],
                                                             start=(k == 0), stop=(k == 7)),
                                 reads=[b_h2Tf[fs], b["wr_f"]], writes=[PB[lb]], inc=(k == 7))
                        K.op(DVE, lambda e: e.tensor_tensor(out=lg[:, tt, :], in0=bank(lb)[:, 0:20], in1=br_f[:], op=ALU.add),
                             reads=[PB[lb], b["br_f"]], writes=[b["lg"]])

                    for i in range(NTP + 2):
                        if i < NTP:
                            s6_st1(i)
                        if 1 <= i <= NTP:
                            s6_st2(i - 1)
                        if i >= 2:
                            s6_st3(i - 2)

                    RW, RS = [b["rw"]], [b["rs"]]

                    def bc(ap2):
                        return ap2.unsqueeze(2).broadcast_to([128, NTP, 4])

                    def vop(fn, reads, writes):
                        K.op(DVE, fn, reads=reads, writes=writes)

                    LGg = lg[:, :, 0:4]
                    gmax, gsum, gp, m1, m2, dd, ee, w1, w2 = [rs[:, i, :] for i in range(9)]
                    goh, gsh, ig, tmp4, oh1, ig2, oh2, within, gw = [rw[:, i, :, :] for i in range(9)]
                    vop(lambda e: e.tensor_reduce(out=gmax, in_=LGg, axis=AX.X, op=ALU.max), [b["lg"]], RS)
                    vop(lambda e: e.tensor_tensor(out=goh, in0=LGg, in1=bc(gmax), op=ALU.is_equal), [b["lg"], b["rs"]], RW)
                    vop(lambda e: e.tensor_tensor(out=gsh, in0=LGg, in1=bc(gmax), op=ALU.subtract), [b["lg"], b["rs"]], RW)
                    K.op(ACT, lambda e: e.activation(out=gsh, in_=gsh, func=AF.Exp), reads=RW, writes=RW)
                    vop(lambda e: e.tensor_reduce(out=gsum, in_=gsh, axis=AX.X, op=ALU.add), RW, RS)
                    vop(lambda e: e.reciprocal(out=gp, in_=gsum), RS, RS)
                    for g in range(4):
                        Lg = lg[:, :, 4 + 4 * g:8 + 4 * g]
                        gsel = rw[:, 0, :, g:g + 1].broadcast_to([128, NTP, 4])
                        if g == 0:
                            vop(lambda e, Lg=Lg, gsel=gsel: e.tensor_tensor(out=ig, in0=Lg, in1=gsel, op=ALU.mult), [b["lg"], b["rw"]], RW)
                        else:
                            vop(lambda e, Lg=Lg, gsel=gsel: e.tensor_tensor(out=tmp4, in0=Lg, in1=gsel, op=ALU.mult), [b["lg"], b["rw"]], RW)
                            vop(lambda e: e.tensor_tensor(out=ig, in0=ig, in1=tmp4, op=ALU.add), RW, RW)
                    vop(lambda e: e.tensor_reduce(out=m1, in_=ig, axis=AX.X, op=ALU.max), RW, RS)
                    vop(lambda e: e.tensor_tensor(out=oh1, in0=ig, in1=bc(m1), op=ALU.is_equal), RW + RS, RW)
                    vop(lambda e: e.scalar_tensor_tensor(out=ig2, in0=oh1, scalar=-1e30, in1=ig, op0=ALU.mult, op1=ALU.add), RW, RW)
                    vop(lambda e: e.tensor_reduce(out=m2, in_=ig2, axis=AX.X, op=ALU.max), RW, RS)
                    vop(lambda e: e.tensor_tensor(out=oh2, in0=ig2, in1=bc(m2), op=ALU.is_equal), RW + RS, RW)
                    vop(lambda e: e.tensor_tensor(out=dd, in0=m2, in1=m1, op=ALU.subtract), RS, RS)
                    K.op(ACT, lambda e: e.activation(out=ee, in_=dd, func=AF.Exp), reads=RS, writes=RS)
                    vop(lambda e: e.tensor_scalar(out=dd, in0=ee, scalar1=1.0, scalar2=None, op0=ALU.add), RS, RS)
                    vop(lambda e: e.reciprocal(out=w1, in_=dd), RS, RS)
                    vop(lambda e: e.tensor_tensor(out=w2, in0=ee, in1=w1, op=ALU.mult), RS, RS)
                    vop(lambda e: e.tensor_tensor(out=within, in0=oh1, in1=bc(w1), op=ALU.mult), RW + RS, RW)
                    vop(lambda e: e.tensor_tensor(out=tmp4, in0=oh2, in1=bc(w2), op=ALU.mult), RW + RS, RW)
                    vop(lambda e: e.tensor_tensor(out=within, in0=within, in1=tmp4, op=ALU.add), RW, RW)
                    vop(lambda e: e.tensor_tensor(out=gw, in0=goh, in1=bc(gp), op=ALU.mult), RW + RS, RW)
                    for g in range(4):
                        gwb = rw[:, 8, :, g:g + 1].broadcast_to([128, NTP, 4])
                        vop(lambda e, g=g, gwb=gwb: e.tensor_tensor(out=comb[:, :, 4 * g:4 * g + 4], in0=within, in1=gwb, op=ALU.mult),
                            RW, [b["comb"]])

                    first_pass = (seq == 0 and p == 0)
                    ecnt = [0]

                    def load_expert(ex, scratch=False):
                        sl = ex % 2
                        if scratch or not first_pass:
                            K.dma(SP, wgb[sl][:].rearrange("p k n -> p (k n)"), wsc_g[ex], None, reads=[b_wsc[ex]], writes=[b_wgb[sl]])
                            K.dma(SP, wubx[sl][:].rearrange("p k n -> p (k n)"), wsc_u[ex], None, reads=[b_wsc[ex]], writes=[b_wubx[sl]])
                            K.dma(SP, wdb[sl][:].rearrange("p k n -> p (k n)"), wsc_d[ex], None, reads=[b_wsc[ex]], writes=[b_wdb[sl]])
                            return
                        stg_t = [estg[0], estg[1], h2f, tmpf2, h2Tf[0], h2Tf[1]]
                        stg_b = [b_estg[0], b_estg[1], b["h2f"], b["tmpf2"], b_h2Tf[0], b_h2Tf[1]]

                        def stg_view(es, k):
                            tt_ = stg_t[es]
                            if es >= 4:
                                return tt_[:].rearrange("p k c -> p (k c)").rearrange("p (k n) -> p k n", k=k)
                            return tt_[:].rearrange("p (k n) -> p k n", k=k)

                        for (src, dstt, dbuf, ce) in ((w_gate, wgb, b_wgb, ACT), (w_up, wubx, b_wubx, POOL)):
                            for i in range(4):
                                es = ecnt[0] % 6
                                ecnt[0] += 1
                                K.dma(SP, stg_view(es, 2),
                                      src[ex, i * 256:(i + 1) * 256, :].rearrange("(k p) n -> p k n", p=128), None, writes=[stg_b[es]])
                                if ce is ACT:
                                    K.op(ce, lambda e, es=es, i=i, dstt=dstt, sl=sl: e.copy(
                                        out=dstt[sl][:, 2 * i:2 * i + 2, :], in_=stg_view(es, 2)),
                                        reads=[stg_b[es]], writes=[dbuf[sl]])
                                else:
                                    K.op(ce, lambda e, es=es, i=i, dstt=dstt, sl=sl: e.tensor_copy(
                                        out=dstt[sl][:, 2 * i:2 * i + 2, :], in_=stg_view(es, 2)),
                                        reads=[stg_b[es]], writes=[dbuf[sl]])
                        for i in range(4):
                            es = ecnt[0] % 6
                            ecnt[0] += 1
                            K.dma(SP, stg_view(es, 1), w_down[ex:ex + 1, i * 128:(i + 1) * 128, :].rearrange("o p n -> p o n"), None, writes=[stg_b[es]])
                            K.op(DVE, lambda e, es=es, i=i, sl=sl: e.tensor_copy(out=wdb[sl][:, i:i + 1, :], in_=stg_view(es, 1)),
                                 reads=[stg_b[es]], writes=[b_wdb[sl]])
                        K.dma(SP, wsc_g[ex], wgb[sl][:].rearrange("p k n -> p (k n)"), None, reads=[b_wgb[sl]], writes=[b_wsc[ex]])
                        K.dma(SP, wsc_u[ex], wubx[sl][:].rearrange("p k n -> p (k n)"), None, reads=[b_wubx[sl]], writes=[b_wsc[ex]])
                        K.dma(SP, wsc_d[ex], wdb[sl][:].rearrange("p k n -> p (k n)"), None, reads=[b_wdb[sl]], writes=[b_wsc[ex]])

                    blocks = [(ex, jb) for ex in range(NE) for jb in range(NBP)]
                    gcnt = [0]
                    ycnt = [0]

                    def stage_a(bi):
                        ex, jb = blocks[bi]
                        sl = ex % 2
                        cs = slice(jb * 512, (jb + 1) * 512)
                        for mch in range(4):
                            gi = gcnt[0] % 2
                            gcnt[0] += 1
                            gb, ub = gi, 2 + gi
                            ai = (bi % 2) * 4 + mch
                            for k in range(8):
                                K.op(PE, lambda e, k=k, sl=sl, mch=mch, gb=gb, cs=cs: e.matmul(
                                    bank(gb), lhsT=wgb[sl][:, k, mch * 128:(mch + 1) * 128], rhs=h2T[:, k, cs],
                                    start=(k == 0), stop=(k == 7)),
                                    reads=[b_wgb[sl], b["h2T"]], writes=[PB[gb]], inc=(k == 7))
                            for k in range(8):
                                K.op(PE, lambda e, k=k, sl=sl, mch=mch, ub=ub, cs=cs: e.matmul(
                                    bank(ub), lhsT=wubx[sl][:, k, mch * 128:(mch + 1) * 128], rhs=h2T[:, k, cs],
                                    start=(k == 0), stop=(k == 7)),
                                    reads=[b_wubx[sl], b["h2T"]], writes=[PB[ub]], inc=(k == 7))
                            K.op(ACT, lambda e, gi=gi, gb=gb: e.activation(out=sg[gi][:], in_=bank(gb), func=AF.Silu),
                                 reads=[PB[gb]], writes=[b_sg[gi]])
                            K.op(DVE, lambda e, gi=gi, ub=ub, ai=ai: e.tensor_tensor(out=aT[ai][:], in0=bank(ub), in1=sg[gi][:], op=ALU.mult),
                                 reads=[PB[ub], b_sg[gi]], writes=[b_aT[ai]])

                    def stage_b(bi):
                        ex, jb = blocks[bi]
                        sl = ex % 2
                        for t4 in range(4):
                            tt = jb * 4 + t4
                            t = p * NTP + tt
                            yi = ycnt[0] % 2
                            yp = 2 + yi
                            ycnt[0] += 1
                            for half in range(2):
                                for mch in range(4):
                                    ai = (bi % 2) * 4 + mch
                                    K.op(PE, lambda e, t4=t4, half=half, mch=mch, ai=ai, yp=yp, sl=sl: e.matmul(
                                        bank(2 * yp + half), lhsT=aT[ai][:, t4 * 128:(t4 + 1) * 128],
                                        rhs=wdb[sl][:, mch, half * 512:(half + 1) * 512], start=(mch == 0), stop=(mch == 3)),
                                        reads=[b_aT[ai], b_wdb[sl]], writes=[PB[2 * yp + half]], inc=(mch == 3))
                            K.op(DVE, lambda e, tt=tt, ex=ex, yp=yp, yi=yi: e.scalar_tensor_tensor(
                                out=ytmp[yi][:], in0=ps[yp][:], scalar=comb[:, tt, ex:ex + 1], in1=G2[:],
                                op0=ALU.mult, op1=ALU.mult),
                                reads=[PB[2 * yp], PB[2 * yp + 1], b["comb"], b["G2"]], writes=[b_ytmp[yi]])
                            K.op(POOL, lambda e, t=t, yi=yi: e.tensor_tensor(out=acc[:, t, :], in0=acc[:, t, :], in1=ytmp[yi][:], op=ALU.add),
                                 reads=[b_acc[t], b_ytmp[yi]], writes=[b_acc[t]])
                        if jb == NBP - 1 and ex + 2 < NE:
                            load_expert(ex + 2)

                    if p == 0:
                        load_expert(0)
                        load_expert(1)
                    for bi in range(len(blocks) + 1):
                        if bi < len(blocks):
                            stage_a(bi)
                        if bi >= 1:
                            stage_b(bi - 1)
                    if p + 1 < NPASS:
                        load_expert(0, scratch=True)
                        load_expert(1, scratch=True)

                    for tt in range(NTP):
                        t = p * NTP + tt
                        d4 = tt % 4
                        ssap = stat[:, 12 + d4:13 + d4]
                        K.op(ACT, lambda e: e.activation(out=junk2[:], in_=acc[:, t, :], func=AF.Square, accum_out=ssap),
                             reads=[b_acc[t]], writes=[b_st[4 + d4]])
                        rstd_from_ss(ssap, 1.0 / D, b_st[4 + d4])
                        K.op(DVE, lambda e: e.scalar_tensor_tensor(out=acc[:, t, :], in0=acc[:, t, :], scalar=ssap, in1=gfin_bc[:],
                                                                   op0=ALU.mult, op1=ALU.mult),
                             reads=[b_acc[t], b_st[4 + d4], b["gfin_bc"]], writes=[b_acc[t]])
                        K.dma(POOL, out[seq, t * 128:(t + 1) * 128, :], acc[:, t, :], s_out, reads=[b_acc[t]])
                K.barrier(new_sems=(seq + 1 < NSEQ))


        except _Stop:
            K.barrier()
            if STOP == 'b2':
                dflat = out[0].rearrange("(p a) d -> p (a d)", p=128)
                for k in range(8):
                    for jj in range(S // 512):
                        K.op(DVE, lambda e, k=k, jj=jj: e.tensor_copy(out=tA[:], in_=oT[:, k, jj * 512:(jj + 1) * 512]),
                             reads=[b_oT[k]], writes=[b["tA"]])
                        K.dma(SP, dflat[:, k * S + jj * 512:k * S + (jj + 1) * 512], tA[:], s_out, reads=[b["tA"]])
            if STOP == 'b3':
                for t in range(NT):
                    K.dma(SP, out[0, t * 128:(t + 1) * 128, :], acc[:, t, :], s_out, reads=[b_acc[t]])
        K.finish([(SP, s_out)])
    return nc


def _consts(S):
    ident = np.eye(128, dtype=np.float32)
    rt = np.zeros((128, 128), dtype=np.float32)
    for p in range(128):
        if p % 64 < 32:
            rt[p + 32, p] = -1.0
        else:
            rt[p - 32, p] = 1.0
    kk = np.arange(128)[:, None]
    qq = np.arange(128)[None, :]
    tri = (kk <= qq).astype(np.float32)
    prev = (qq < kk).astype(np.float32)
    swam = np.concatenate([prev, tri], axis=1)
    inv = (10000.0 ** (-np.arange(0, 64, 2, dtype=np.float32) / np.float32(64))).astype(np.float32)
    ang = np.arange(S, dtype=np.float32)[:, None] * inv[None, :]
    ang = np.concatenate([ang, ang], axis=-1)
    cos = np.cos(ang).astype(np.float32).T
    sin = np.sin(ang).astype(np.float32).T
    cosT = np.concatenate([cos, cos], axis=0).astype(ml_dtypes.bfloat16)
    sinT = np.concatenate([sin, sin], axis=0).astype(ml_dtypes.bfloat16)
    return ident, rt, tri, swam, np.ascontiguousarray(cosT), np.ascontiguousarray(sinT)


_NC_CACHE = {}


def kernel(x, c, w_ada, b_ada, g_mix, w_in, diff_lambda, g_diff_sub, swa_sinks, w_out,
           g_ffn, w_route_group, b_route_group, w_route_expert, b_route_expert,
           w_gate, w_up, w_down, g_final):
    x = np.asarray(x, dtype=np.float32)
    B, S, _ = x.shape
    NSEQ = B // NCORES
    f = lambda a: np.ascontiguousarray(np.asarray(a, dtype=np.float32))
    w_in0 = f(w_in)[0]
    cols = []
    for h in range(4):
        cols.append(w_in0[:, h * 128:(h + 1) * 128])
        cols.append(w_in0[:, 512 + h * 128:512 + (h + 1) * 128])
    for ch in range(4):
        hk = ch // 2
        cols.append(w_in0[:, 1536 + ch * 128:1536 + (ch + 1) * 128])
        skh = w_in0[:, 2048 + hk * 64:2048 + (hk + 1) * 64]
        cols.append(skh)
        cols.append(skh)
    cols.append(w_in0[:, 1024:1536])
    cols.append(w_in0[:, 2176:2304])
    w_in_ext = np.ascontiguousarray(np.concatenate(cols, axis=1))
    assert w_in_ext.shape == (1024, 2688)
    sinks = f(swa_sinks)[0]
    sink_c = np.zeros((128, 4), np.float32)
    for ch in range(4):
        sink_c[0:64, ch] = sinks[2 * ch]
        sink_c[64:128, ch] = sinks[2 * ch + 1]
    ident, rt, tri, swam, cosT, sinT = _consts(S)
    w_r = np.ascontiguousarray(np.concatenate([f(w_route_group)[0], f(w_route_expert)[0]], axis=1))
    b_r = np.ascontiguousarray(np.concatenate([f(b_route_group)[0], f(b_route_expert)[0]])[None, :])
    shared = dict(
        w_ada=f(w_ada)[0], b_ada=f(b_ada), g_mix=f(g_mix), g_ffn=f(g_ffn), g_fin=f(g_final)[None, :],
        w_in=w_in_ext, dlam=f(diff_lambda).reshape(1, 256), gds_c=f(g_diff_sub).reshape(128, 1), sink_c=sink_c,
        w_out=f(w_out)[0], w_r=w_r, b_r=b_r, w_gate=f(w_gate)[0], w_up=f(w_up)[0], w_down=f(w_down)[0],
        c_ident=ident, c_rt=rt, c_tri=tri, c_swam=swam, c_cos=cosT, c_sin=sinT,
    )
    cf = f(c)
    in_maps = []
    for i in range(NCORES):
        m = dict(shared)
        m["x"] = np.ascontiguousarray(x[i * NSEQ:(i + 1) * NSEQ])
        cc = cf[i * NSEQ:(i + 1) * NSEQ]
        m["cT"] = np.ascontiguousarray(cc.reshape(NSEQ, 8, 128).transpose(2, 1, 0))
        in_maps.append(m)
    key = (NSEQ, S)
    if key not in _NC_CACHE:
        _NC_CACHE[key] = build_nc(NSEQ, S)
    nc = _NC_CACHE[key]
    res = run_bass_kernel_spmd(nc, in_maps, core_ids=list(range(NCORES)))
    outs = [np.asarray(r["out"], dtype=np.float32).reshape(NSEQ, S, D) for r in res.results]
    return np.concatenate(outs, axis=0)
```
